# Optimizing a Trainium2 kernel written in Bass

```python
import jax, jax.numpy as jnp
from jax import lax
import numpy as np

D_MODEL = 1024
BATCH = 32
SEQ = 2048
DEPTH = 2
DEC_BATCH = 16
DEC_SEQ = 2048
PAST_LEN = 128

D_CONV = 512
CONV_WIDTH = 3
HEAD_DIM = 64
HEADS_PER_GROUP = 8
ATTN_GROUPS = ((128, 1), (512, 4), (2048, 16))
N_GROUPS = 3
ATTN_WIDTH = HEADS_PER_GROUP * HEAD_DIM
QKV_WIDTH = N_GROUPS * ATTN_WIDTH
ROPE_DIM = HEAD_DIM // 4
ROPE_THETA = 500000.0
N_EXPERTS = 16
D_EXPERT = 2048
CAPACITY_FACTOR = 2
RMS_EPS = 1e-6
NEG_INF = -1e30
OFF_B = 0
OFF_C = D_CONV
OFF_H = 2 * D_CONV
OFF_Q = 3 * D_CONV
OFF_K = OFF_Q + QKV_WIDTH
OFF_V = OFF_K + QKV_WIDTH
OFF_G = OFF_V + QKV_WIDTH
D_IN = OFF_G + 2 * D_MODEL

kernel_name = "hybrid_conv_dilated_attn_ec_moe_encoder"


def rms_norm(x, g):
    xf = x.astype(jnp.float32)
    y = xf * lax.rsqrt(jnp.mean(xf * xf, axis=-1, keepdims=True) + RMS_EPS)
    return (y * g.astype(jnp.float32)).astype(x.dtype)


def partial_rotary(t, pos):
    half = ROPE_DIM // 2
    inv = jnp.power(ROPE_THETA, -jnp.arange(half, dtype=jnp.float32) * (2.0 / ROPE_DIM))
    ang = pos.astype(jnp.float32)[:, None] * inv[None, :]
    cos = jnp.cos(ang)[None, :, None, :]
    sin = jnp.sin(ang)[None, :, None, :]
    tr = t[..., :ROPE_DIM].astype(jnp.float32)
    t1, t2 = tr[..., :half], tr[..., half:]
    rot = jnp.concatenate([t1 * cos - t2 * sin, t2 * cos + t1 * sin], axis=-1).astype(t.dtype)
    return jnp.concatenate([rot, t[..., ROPE_DIM:]], axis=-1)


def short_conv(u, w):
    up = jnp.pad(u, ((0, 0), (1, 1), (0, 0)))
    w = w.astype(u.dtype)
    return up[:, :-2] * w[0] + up[:, 1:-1] * w[1] + up[:, 2:] * w[2]


def dilated_window_attention(q, k, v, window, dilation):
    B, S, H, Dh = q.shape
    R = window // (2 * dilation)
    QB = R
    L = S // dilation
    nb = -(-L // QB)
    Lp = nb * QB
    Z = B * dilation

    def to_classes(t):
        t = t.reshape(B, L, dilation, H, Dh).transpose(0, 2, 1, 3, 4)
        return t.reshape(Z, L, H, Dh)

    qc, kc, vc = to_classes(q), to_classes(k), to_classes(v)
    qb = jnp.pad(qc, ((0, 0), (0, Lp - L), (0, 0), (0, 0))).reshape(Z, nb, QB, H, Dh)

    def key_windows(t):
        tp = jnp.pad(t, ((0, 0), (QB, Lp - L + QB), (0, 0), (0, 0))).reshape(Z, nb + 2, QB, H, Dh)
        return jnp.concatenate([tp[:, :-2], tp[:, 1:-1], tp[:, 2:]], axis=2)

    kw, vw = key_windows(kc), key_windows(vc)
    s = jnp.einsum('znqhd,znkhd->znhqk', qb, kw,
                   preferred_element_type=jnp.float32) * (Dh ** -0.5)
    qpos = jnp.arange(nb)[:, None] * QB + jnp.arange(QB)[None, :]
    kpos = (jnp.arange(nb)[:, None] - 1) * QB + jnp.arange(3 * QB)[None, :]
    valid = (jnp.abs(qpos[:, :, None] - kpos[:, None, :]) <= R) \
        & ((kpos >= 0) & (kpos < L))[:, None, :]
    s = jnp.where(valid[None, :, None], s, NEG_INF)
    m = jnp.max(s, axis=-1, keepdims=True)
    p = jnp.exp(s - m)
    den = jnp.sum(p, axis=-1)
    o = jnp.einsum('znhqk,znkhd->znqhd', p, vw.astype(jnp.float32))
    o = o / jnp.transpose(den, (0, 1, 3, 2))[..., None]
    lse = jnp.transpose(m[..., 0] + jnp.log(den), (0, 1, 3, 2))
    o = o.reshape(Z, Lp, H, Dh)[:, :L].reshape(B, dilation, L, H, Dh)
    o = o.transpose(0, 2, 1, 3, 4).reshape(B, S, H, Dh)
    lse = lse.reshape(Z, Lp, H)[:, :L].reshape(B, dilation, L, H).transpose(0, 2, 1, 3).reshape(B, S, H)
    return o, lse


def expert_choice_ffn(h, w_router, w_gate, w_up, w_down):
    B, S, D = h.shape
    T = B * S
    cap = CAPACITY_FACTOR * T // N_EXPERTS
    hf = h.reshape(T, D)
    aff = jax.nn.softmax((hf @ w_router).astype(jnp.float32), axis=-1)
    gate, idx = lax.top_k(aff.T, cap)
    xs = hf[idx]
    a = jnp.einsum('ecd,edf->ecf', xs, w_gate)
    u = jnp.einsum('ecd,edf->ecf', xs, w_up)
    y = jnp.einsum('ecf,efd->ecd', jax.nn.silu(a) * u, w_down)
    y = y * gate[..., None].astype(y.dtype)
    out = jnp.zeros((T, D), h.dtype).at[idx.reshape(-1)].add(y.reshape(-1, D).astype(h.dtype))
    return out.reshape(B, S, D)


def hybrid_layer(x, n1, w_in, conv_w, w_conv_out, w_attn_out, w_o, n2, w_router, w_gate, w_up, w_down):
    B, S, _ = x.shape
    pos = jnp.arange(S)
    h = rms_norm(x, n1)
    z = h @ w_in
    b_g = z[..., OFF_B:OFF_C]
    c_g = z[..., OFF_C:OFF_H]
    h_c = z[..., OFF_H:OFF_Q]
    conv_branch = (b_g * short_conv(c_g * h_c, conv_w)) @ w_conv_out
    q = z[..., OFF_Q:OFF_K].reshape(B, S, N_GROUPS * HEADS_PER_GROUP, HEAD_DIM)
    k = z[..., OFF_K:OFF_V].reshape(B, S, N_GROUPS * HEADS_PER_GROUP, HEAD_DIM)
    v = z[..., OFF_V:OFF_G].reshape(B, S, N_GROUPS * HEADS_PER_GROUP, HEAD_DIM)
    q = partial_rotary(q, pos)
    k = partial_rotary(k, pos)
    outs, lses = [], []
    for g, (win, dil) in enumerate(ATTN_GROUPS):
        sl = slice(g * HEADS_PER_GROUP, (g + 1) * HEADS_PER_GROUP)
        o_g, l_g = dilated_window_attention(q[:, :, sl], k[:, :, sl], v[:, :, sl], win, dil)
        outs.append(o_g)
        lses.append(l_g)
    alpha = jax.nn.softmax(jnp.stack(lses, axis=0), axis=0)
    o = jnp.sum(alpha[..., None] * jnp.stack(outs, axis=0), axis=0)
    attn_branch = o.astype(x.dtype).reshape(B, S, ATTN_WIDTH) @ w_attn_out
    gates = jax.nn.sigmoid(z[..., OFF_G:].astype(jnp.float32)).astype(x.dtype)
    merged = gates[..., :D_MODEL] * conv_branch + gates[..., D_MODEL:] * attn_branch
    x = x + merged @ w_o
    x = x + expert_choice_ffn(rms_norm(x, n2), w_router, w_gate, w_up, w_down)
    return x


def trunk(x, norm1_g, w_in, conv_w, w_conv_out, w_attn_out, w_o, norm2_g, w_router, w_gate, w_up, w_down, final_g):
    for l in range(DEPTH):
        x = hybrid_layer(x, norm1_g[l], w_in[l], conv_w[l], w_conv_out[l], w_attn_out[l], w_o[l],
                         norm2_g[l], w_router[l], w_gate[l], w_up[l], w_down[l])
    return rms_norm(x, final_g)


def setup_inputs(seed: int = 0) -> dict:
    key = jax.random.key(seed)
    ks = jax.random.split(key, 16)
    f32 = jnp.float32
    nrm = lambda k, shape, scale: jax.random.normal(k, shape, f32) * scale
    return {
        "x_prompt": nrm(ks[0], (BATCH, SEQ, D_MODEL), 1.0),
        "x_sample": nrm(ks[1], (DEC_BATCH, DEC_SEQ, D_MODEL), 1.0),
        "norm1_g": 1.0 + nrm(ks[2], (DEPTH, D_MODEL), 0.02),
        "w_in": nrm(ks[3], (DEPTH, D_MODEL, D_IN), D_MODEL ** -0.5),
        "conv_w": nrm(ks[4], (DEPTH, CONV_WIDTH, D_CONV), CONV_WIDTH ** -0.5),
        "w_conv_out": nrm(ks[5], (DEPTH, D_CONV, D_MODEL), D_CONV ** -0.5),
        "w_attn_out": nrm(ks[6], (DEPTH, ATTN_WIDTH, D_MODEL), ATTN_WIDTH ** -0.5),
        "w_o": nrm(ks[7], (DEPTH, D_MODEL, D_MODEL), D_MODEL ** -0.5),
        "norm2_g": 1.0 + nrm(ks[8], (DEPTH, D_MODEL), 0.02),
        "w_router": nrm(ks[9], (DEPTH, D_MODEL, N_EXPERTS), D_MODEL ** -0.5),
        "w_gate": nrm(ks[10], (DEPTH, N_EXPERTS, D_MODEL, D_EXPERT), D_MODEL ** -0.5),
        "w_up": nrm(ks[11], (DEPTH, N_EXPERTS, D_MODEL, D_EXPERT), D_MODEL ** -0.5),
        "w_down": nrm(ks[12], (DEPTH, N_EXPERTS, D_EXPERT, D_MODEL), D_EXPERT ** -0.5),
        "final_g": 1.0 + nrm(ks[13], (D_MODEL,), 0.02),
    }


def reference(x_prompt, x_sample, norm1_g, w_in, conv_w, w_conv_out, w_attn_out, w_o, norm2_g,
              w_router, w_gate, w_up, w_down, final_g):
    y_prompt = trunk(x_prompt, norm1_g, w_in, conv_w, w_conv_out, w_attn_out, w_o, norm2_g,
                     w_router, w_gate, w_up, w_down, final_g)
    y_sample = trunk(x_sample, norm1_g, w_in, conv_w, w_conv_out, w_attn_out, w_o, norm2_g,
                     w_router, w_gate, w_up, w_down, final_g)
    return (y_prompt, y_sample)
```

```python
import numpy as np
from contextlib import ExitStack
import concourse.bass as bass
import concourse.mybir as mybir
from concourse.bass_utils import run_bass_kernel_spmd

F32 = mybir.dt.float32
BF16 = mybir.dt.bfloat16
I32 = mybir.dt.int32
ALU = mybir.AluOpType
AF = mybir.ActivationFunctionType
AX = mybir.AxisListType

NCORE = 8
D = 1024
S_LEN = 2048
DEPTH = 2
D_IN = 8192
OFF_Q, OFF_K, OFF_V, OFF_G = 1536, 3072, 4608, 6144
NE = 16
DE = 2048
PAD = 256
GROUPS = ((1, 2048), (4, 512), (16, 128))
RECW = 1056
EPS = 1e-6


class Buf:
    def __init__(self, name):
        self.name = name
        self.w = {}
        self.r = {}
        self.dsem = None
        self.dcnt = 0


class StopBuild(Exception):
    pass


class Sched:
    nops = 0
    stop_at = None

    def __init__(self, nc, stack):
        self.nc = nc
        self.stack = stack
        self.eng = {"pe": nc.tensor, "act": nc.scalar, "dve": nc.vector,
                    "pool": nc.gpsimd, "sp": nc.sync}
        self.sem, self.cnt, self.known, self.allsems = {}, {}, {}, {}
        for k in self.eng:
            s = stack.enter_context(nc.semaphore("s_" + k))
            self.sem[k] = s
            self.cnt[k] = 0
            self.known[k] = {}
            self.allsems["s_" + k] = [s, 0]
        self.nbuf = 0
        self.bufs = []
        self.free_dsems = []

    def buf(self, name):
        self.nbuf += 1
        b = Buf("%s_%d" % (name, self.nbuf))
        self.bufs.append(b)
        return b

    def mark(self):
        return len(self.bufs)

    def release_since(self, mark):
        for b in self.bufs[mark:]:
            if b.dsem is not None:
                self.free_dsems.append((b.dsem, b.dcnt, b.dkey))
                b.dsem = None
        del self.bufs[mark:]

    @staticmethod
    def _need(needs, evs):
        for k, (s, v) in evs.items():
            if k not in needs or needs[k][1] < v:
                needs[k] = (s, v)

    def _waits(self, e, needs):
        eng = self.eng[e]
        kn = self.known[e]
        for k, (s, v) in needs.items():
            if e == "pe" and k == "s_pe":
                continue
            if kn.get(k, 0) < v:
                eng.wait_ge(s, v)
                kn[k] = v

    def op(self, e, fn, reads=(), writes=(), dma=None, inc=16):
        self.nops += 1
        if self.stop_at is not None and self.nops > self.stop_at:
            return {}
        needs = {}
        for b in reads:
            self._need(needs, b.w)
        for b in writes:
            self._need(needs, b.w)
            self._need(needs, b.r)
        self._waits(e, needs)
        ins = fn()
        if dma is not None:
            if dma.dsem is None:
                if self.free_dsems:
                    dma.dsem, dma.dcnt, dma.dkey = self.free_dsems.pop()
                else:
                    dma.dsem = self.stack.enter_context(self.nc.semaphore("d_" + dma.name))
                    dma.dkey = "d_" + dma.name
                    dma.dcnt = 0
                    self.allsems[dma.dkey] = [dma.dsem, 0]
            dma.dcnt += inc
            if inc == 1:
                ins.then_inc(dma.dsem)
            else:
                ins.then_inc(dma.dsem, inc)
            key = dma.dkey
            ev = (dma.dsem, dma.dcnt)
        else:
            self.cnt[e] += 1
            ins.then_inc(self.sem[e], 1)
            key = "s_" + e
            ev = (self.sem[e], self.cnt[e])
        self.allsems[key][1] = ev[1]
        for b in writes:
            b.w = {key: ev}
            b.r = {}
        for b in reads:
            if b in writes:
                continue
            b.r[key] = ev
        return {key: ev}

    def barrier(self):
        if self.stop_at is not None and self.nops > self.stop_at:
            if getattr(self, "_final", False):
                return
            self._final = True
        needs = {k: (s, v) for k, (s, v) in self.allsems.items() if v > 0}
        for e in self.eng:
            self._waits(e, needs)


def make_consts():
    p = np.arange(128)
    ident = np.eye(128, dtype=np.float32)
    gsel = (p[:, None] % 16 == p[None, :] % 16).astype(np.float32)
    iota = p.astype(np.float32)[:, None]
    caps = np.zeros((128, 2), np.float32)
    i16 = np.zeros((128, 16), np.float32)
    i16[:16] = np.eye(16)
    half = 8
    inv = np.power(np.float32(500000.0), -np.arange(half, dtype=np.float32) * np.float32(2.0 / 16))
    ang = np.arange(S_LEN, dtype=np.float32)[None, :] * inv[:, None]
    cosT = np.ones((128, S_LEN), np.float32)
    sinT = np.zeros((128, S_LEN), np.float32)
    pt = np.zeros((128, 128), np.float32)
    for h0 in (0, 64):
        for dd in range(16):
            cosT[h0 + dd] = np.cos(ang[dd % 8])
            sinT[h0 + dd] = np.sin(ang[dd % 8])
        for m in range(8):
            pt[h0 + m + 8, h0 + m] = -1.0
            pt[h0 + m, h0 + m + 8] = 1.0
    i = p[:, None]
    c = p[None, :]
    A = (c <= i).astype(np.float32)
    B = (c >= i).astype(np.float32)
    Af = A * (i >= 64)
    Bl = B * (i < 64)
    C = (np.abs(c - i) <= 64).astype(np.float32)
    masks = np.concatenate([Af, B, A, B, A, B, A, B, A, B, A, Bl, C, C, C, C], 1)
    U = (p[:, None] < p[None, :]).astype(np.float32)
    ones = np.ones((128, 128), np.float32)
    cstf = np.concatenate([ident, gsel, iota, i16], 1)
    cstb = np.concatenate([ident, pt, U, ones, masks, cosT, sinT], 1)
    return np.ascontiguousarray(cstf), np.ascontiguousarray(cstb)


def build(nP, nS, cap_slots, dbg=None, stop=None):
    NSEQ = nP + nS
    NT = NSEQ * S_LEN
    NTILE = NT // 128
    nPT = nP * 16
    CAPT = cap_slots // 128
    capP = 2 * (nP * NCORE * S_LEN) // NE
    capS = 2 * (nS * NCORE * S_LEN) // NE
    nc = bass.Bass("TRN2", target_bir_lowering=False)

    tiny = (dbg == "Bonly")

    def din(name, shape):
        if tiny and name.startswith("w_"):
            shape = [1] * len(shape)
        return nc.dram_tensor(name, shape, F32, kind="ExternalInput").ap()

    x_in = din("x_in", [NT, D])
    norm1_g = din("norm1_g", [DEPTH, D])
    w_in = din("w_in", [DEPTH, D, D_IN])
    conv_w = din("conv_w", [DEPTH, 3, 512])
    w_conv_out = din("w_conv_out", [DEPTH, 512, D])
    w_attn_out = din("w_attn_out", [DEPTH, 512, D])
    w_o = din("w_o", [DEPTH, D, D])
    norm2_g = din("norm2_g", [DEPTH, D])
    w_router = din("w_router", [DEPTH, D, NE])
    w_gate = din("w_gate", [DEPTH, NE, D, DE])
    w_up = din("w_up", [DEPTH, NE, D, DE])
    w_down = din("w_down", [DEPTH, NE, DE, D])
    final_g = din("final_g", [1, D])
    cstf_d = din("cstf", [128, 273])
    cstb_d = din("cstb", [128, 512 + 2048 + 4096])
    xs_init = din("xs_init", [cap_slots, RECW])
    capv_d = din("capv", [128, 2])
    y_out = nc.dram_tensor("y", [NT, D], F32, kind="ExternalOutput").ap()
    if dbg is not None:
        y1_out = nc.dram_tensor("y1", [NT, D], F32, kind="ExternalOutput").ap()
        dbg_out = nc.dram_tensor("dbgo", [128, 64], F32, kind="ExternalOutput").ap()

    xbuf = nc.dram_tensor("xbuf", [NT + 128, D], F32).ap()
    h2rec = nc.dram_tensor("h2rec", [NT, RECW], F32).ap()
    xs_d = [nc.dram_tensor("xs_d%d" % e, [cap_slots, RECW], F32).ap() for e in range(NE)]
    affT_d = nc.dram_tensor("affT_d", [NE, NT], F32).ap()
    gath_d = nc.dram_tensor("gath_d", [128, NT], F32).ap()

    with ExitStack() as st:
      S = Sched(nc, st)
      if isinstance(stop, int):
          S.stop_at = stop
      try:

            uid = [0]

            def sb(stack, name, shape, dt=F32):
                uid[0] += 1
                return stack.enter_context(nc.sbuf_tensor("sb_%s_%d" % (name, uid[0]), shape, dt))

            banks = [st.enter_context(nc.psum_tensor("bank%d" % i, [128, 512], F32)) for i in range(7)]
            BK = [S.buf("bank%d" % i) for i in range(7)]
            bankb = st.enter_context(nc.psum_tensor("bankb", [128, 1024], BF16))
            BKB = S.buf("bankb")
            rr = [0]

            def nextbank(lo=0, hi=3):
                i = lo + rr[0] % (hi - lo)
                rr[0] += 1
                return banks[i], BK[i]

            cstf = sb(st, "cstf", [128, 273]); CSTF = S.buf("cstf")
            cb = sb(st, "cb", [128, 512 + 2048 + 4096], BF16); CB = S.buf("cb")
            S.op("sp", lambda: nc.sync.dma_start(out=cstf[:], in_=cstf_d[:, :]), writes=[CSTF], dma=CSTF)
            for q in range(0, 6656, 1664):
                S.op("pool", lambda q=q: nc.gpsimd.dma_start(out=cb[:, q:q + 1664], in_=cstb_d[:, q:q + 1664]),
                     writes=[CB], dma=CB)
            identf = cstf[:, 0:128]
            gsel = cstf[:, 128:256]
            iota = cstf[:, 256:257]
            i16 = cstf[0:16, 257:273]
            identb = cb[:, 0:128]
            ptb = cb[:, 128:256]
            Ub = cb[:, 256:384]
            onesb = cb[:, 384:512]
            maskb = [cb[:, 512 + 512 * k: 1024 + 512 * k] for k in range(4)]
            cosb = cb[:, 2560:2560 + 2048]
            sinb = cb[:, 4608:4608 + 2048]
            capv = sb(st, "capv", [128, 2]); CAPV = S.buf("capv")
            S.op("sp", lambda: nc.sync.dma_start(out=capv[:], in_=capv_d[:, :]), writes=[CAPV], dma=CAPV)

            aff_all = sb(st, "aff_all", [128, NTILE, NE]); AFF = S.buf("aff")
            small = sb(st, "small", [128, 64]); SM = [S.buf("sm%d" % i) for i in range(64)]
            wrr = [0]

            bc_slots = nc.gpsimd.to_reg(cap_slots - 1)
            bc_rows = nc.gpsimd.to_reg(NT + 127)
            XB = [S.buf("xbuf%d" % s) for s in range(NSEQ)]
            H2R = S.buf("h2rec")
            XIN = S.buf("xin")

            def wv(l):
                return w_in[l].rearrange("(kc k) c -> k kc c", k=128)

            def rstd_ops(ss, rs, SSB, RSB):
                S.op("dve", lambda: nc.vector.tensor_scalar(out=rs, in0=ss, scalar1=1.0 / D, scalar2=EPS,
                                                             op0=ALU.mult, op1=ALU.add), reads=[SSB], writes=[RSB])
                S.op("act", lambda: nc.scalar.activation(out=rs, in_=rs, func=AF.Sqrt), reads=[RSB], writes=[RSB])
                S.op("dve", lambda: nc.vector.reciprocal(out=rs, in_=rs), reads=[RSB], writes=[RSB])

            for l in range(DEPTH):
                if tiny:
                    with ExitStack() as la:
                        xt0 = sb(la, "xt0", [128, NE]); XT0 = S.buf("xt0")
                        for gt in range(NTILE):
                            S.op("sp", lambda gt=gt: nc.sync.dma_start(out=xt0[:], in_=x_in[gt * 128:(gt + 1) * 128, 0:NE]), writes=[XT0], dma=XT0)
                            S.op("act", lambda gt=gt: nc.scalar.activation(out=aff_all[:, gt, :], in_=xt0[:], func=AF.Sigmoid), reads=[XT0], writes=[AFF])
                        S.barrier()
                mk_layer = S.mark()
                with ExitStack() as la:
                  if not tiny:
                    gbc = sb(la, "gbc", [128, D]); GBC = S.buf("gbc")
                    g2bc = sb(la, "g2bc", [128, D]); G2BC = S.buf("g2bc")
                    wbufs = [sb(la, "wb%d" % i, [128, 8, 3, 128], BF16) for i in range(2)]
                    WB = [S.buf("wb%d" % i) for i in range(2)]

                    def nextw():
                        i = wrr[0] % 2
                        wrr[0] += 1
                        return wbufs[i], WB[i]

                    hT = sb(la, "hT", [128, 8, S_LEN + 2 * PAD], BF16)
                    HT = [S.buf("hT%d" % i) for i in range(16)]
                    HTP = S.buf("hTpad")
                    ucT = sb(la, "ucT", [128, 4, S_LEN], BF16); UCT = [S.buf("ucT%d" % j) for j in range(4)]
                    oT = sb(la, "oT", [128, 4, S_LEN], BF16); OT = [S.buf("oT%d" % j) for j in range(4)]
                    wo = sb(la, "wo", [128, 8, D], BF16); WO = S.buf("wo")
                    wrg = sb(la, "wrg", [128, 8, NE]); WRG = S.buf("wrg")
                    cw = sb(la, "cw", [128, 3, 4]); CW = S.buf("cw")
                    xts = [sb(la, "xt%d" % i, [128, D]) for i in range(2)]
                    XT = [S.buf("xt%d" % i) for i in range(2)]
                    junk = sb(la, "junk", [128, D], BF16); JUNK = S.buf("junk")

                    S.op("pool", lambda: nc.gpsimd.memset(hT[:, :, 0:PAD], 0.0), writes=[HTP])
                    S.op("pool", lambda: nc.gpsimd.memset(hT[:, :, PAD + S_LEN:], 0.0), writes=[HTP])
                    for kc in range(8):
                        S.op("pool", lambda kc=kc: nc.gpsimd.dma_start(out=wo[:, kc, :], in_=w_o[l, kc * 128:(kc + 1) * 128, :]),
                             writes=[WO], dma=WO)
                    S.op("sp", lambda: nc.sync.dma_start(out=gbc[:], in_=norm1_g[l:l + 1, :].to_broadcast([128, D])),
                         writes=[GBC], dma=GBC)
                    S.op("sp", lambda: nc.sync.dma_start(out=g2bc[:], in_=norm2_g[l:l + 1, :].to_broadcast([128, D])),
                         writes=[G2BC], dma=G2BC)
                    with nc.allow_non_contiguous_dma(reason="tiny per-layer vectors"):
                        for w3 in range(3):
                            S.op("sp", lambda w3=w3: nc.sync.dma_start(out=cw[:, w3, :], in_=conv_w[l, w3].rearrange("(j c) -> c j", c=128)),
                                 writes=[CW], dma=CW)
                        S.op("sp", lambda: nc.sync.dma_start(out=wrg[:], in_=w_router[l].rearrange("(kc k) e -> k kc e", k=128)),
                             writes=[WRG], dma=WRG)
                        g2col = sb(la, "g2col", [128, 8]); G2C = S.buf("g2col")
                        S.op("sp", lambda: nc.sync.dma_start(out=g2col[:], in_=norm2_g[l].rearrange("(kc k) -> k kc", k=128)),
                             writes=[G2C], dma=G2C)
                    for kc in range(8):
                        S.op("dve", lambda kc=kc: nc.vector.tensor_scalar(out=wrg[:, kc, :], in0=wrg[:, kc, :],
                                                                           scalar1=g2col[:, kc:kc + 1], scalar2=None, op0=ALU.mult),
                             reads=[G2C], writes=[WRG])

                    for s in range(NSEQ):
                        mk_seq = S.mark()
                        r0 = s * S_LEN
                        xsrc = x_in if l == 0 else xbuf
                        XSRC = XIN if l == 0 else XB[s]
                        with ExitStack() as ph:
                            hbs = [sb(ph, "hb%d" % i, [128, D], BF16) for i in range(2)]
                            HB = [S.buf("hb%d" % i) for i in range(2)]
                            for tt in range(16):
                                xt, XTb = xts[tt % 2], XT[tt % 2]
                                hb, HBb = hbs[tt % 2], HB[tt % 2]
                                ss, rs = small[:, 0:1], small[:, 1:2]
                                S.op("sp", lambda: nc.sync.dma_start(out=xt[:], in_=xsrc[r0 + tt * 128: r0 + (tt + 1) * 128, :]),
                                     reads=[XSRC], writes=[XTb], dma=XTb)
                                S.op("act", lambda: nc.scalar.activation(out=junk[:], in_=xt[:], func=AF.Square, accum_out=ss),
                                     reads=[XTb], writes=[JUNK, SM[0]])
                                rstd_ops(ss, rs, SM[0], SM[1])
                                S.op("dve", lambda: nc.vector.scalar_tensor_tensor(out=hb[:], in0=xt[:], scalar=rs, in1=gbc[:],
                                                                                    op0=ALU.mult, op1=ALU.mult),
                                     reads=[XTb, SM[1], GBC], writes=[HBb])
                                for kc in range(8):
                                    S.op("pe", lambda kc=kc: nc.tensor.transpose(out=bankb[:, kc * 128:(kc + 1) * 128],
                                                                                  in_=hb[:, kc * 128:(kc + 1) * 128], identity=identb),
                                         reads=[HBb, CB], writes=[BKB])
                                S.op("act", lambda: nc.scalar.copy(out=hT[:, :, PAD + tt * 128: PAD + (tt + 1) * 128],
                                                                   in_=bankb[:, :].rearrange("p (k t) -> p k t", k=8)),
                                     reads=[BKB], writes=[HT[tt]])
                        S.barrier()
                        HTall = HT + [HTP]
                        with ExitStack() as ph:
                            zc = sb(ph, "zc", [128, 3, S_LEN], BF16); ZC = [S.buf("zc%d" % i) for i in range(3)]
                            u = sb(ph, "u", [128, S_LEN + 2]); UU = S.buf("u")
                            yv = sb(ph, "yv", [128, S_LEN]); YV = S.buf("yv")
                            S.op("pool", lambda: nc.gpsimd.memset(u[:, 0:1], 0.0), writes=[UU])
                            S.op("pool", lambda: nc.gpsimd.memset(u[:, S_LEN + 1:S_LEN + 2], 0.0), writes=[UU])
                            for j in range(4):
                                wt, WTb = nextw()
                                for t3 in range(3):
                                    c0 = t3 * 512 + j * 128
                                    S.op("pool", lambda t3=t3, c0=c0: nc.gpsimd.dma_start(out=wt[:, :, t3, :], in_=wv(l)[:, :, c0:c0 + 128]),
                                         writes=[WTb], dma=WTb)
                                for t3 in range(3):
                                    for tc in range(4):
                                        bk, BKb = nextbank()
                                        for kc in range(8):
                                            S.op("pe", lambda kc=kc, t3=t3, tc=tc, bk=bk: nc.tensor.matmul(
                                                bk[:, :], lhsT=wt[:, kc, t3, :], rhs=hT[:, kc, PAD + tc * 512: PAD + (tc + 1) * 512],
                                                start=(kc == 0), stop=(kc == 7)), reads=[WTb] + HTall, writes=[BKb])
                                        S.op("act", lambda t3=t3, tc=tc, bk=bk: nc.scalar.copy(out=zc[:, t3, tc * 512:(tc + 1) * 512], in_=bk[:, :]),
                                             reads=[BKb], writes=[ZC[t3]])
                                S.op("dve", lambda: nc.vector.tensor_tensor(out=u[:, 1:S_LEN + 1], in0=zc[:, 1, :], in1=zc[:, 2, :], op=ALU.mult),
                                     reads=[ZC[1], ZC[2]], writes=[UU])
                                S.op("dve", lambda j=j: nc.vector.tensor_scalar(out=yv[:], in0=u[:, 1:S_LEN + 1], scalar1=cw[:, 1, j:j + 1],
                                                                                 scalar2=None, op0=ALU.mult), reads=[UU, CW], writes=[YV])
                                S.op("dve", lambda j=j: nc.vector.scalar_tensor_tensor(out=yv[:], in0=u[:, 0:S_LEN], scalar=cw[:, 0, j:j + 1],
                                                                                        in1=yv[:], op0=ALU.mult, op1=ALU.add),
                                     reads=[UU, CW], writes=[YV])
                                S.op("dve", lambda j=j: nc.vector.scalar_tensor_tensor(out=yv[:], in0=u[:, 2:S_LEN + 2], scalar=cw[:, 2, j:j + 1],
                                                                                        in1=yv[:], op0=ALU.mult, op1=ALU.add),
                                     reads=[UU, CW], writes=[YV])
                                S.op("dve", lambda j=j: nc.vector.tensor_tensor(out=ucT[:, j, :], in0=zc[:, 0, :], in1=yv[:], op=ALU.mult),
                                     reads=[ZC[0], YV], writes=[UCT[j]])
                        S.barrier()
                        with ExitStack() as ph:
                            accn = sb(ph, "accn", [64, 2, S_LEN]); ACCN = [S.buf("accn%d" % i) for i in range(2)]
                            accd = sb(ph, "accd", [64, 2, S_LEN]); ACCD = [S.buf("accd%d" % i) for i in range(2)]
                            qk = sb(ph, "qk", [128, 2, S_LEN + 2 * PAD], BF16); QK = [S.buf("qk%d" % i) for i in range(2)]
                            qraw = [sb(ph, "qraw%d" % i, [128, 512], BF16) for i in range(2)]
                            QR = [S.buf("qraw%d" % i) for i in range(2)]
                            rt1 = sb(ph, "rt1", [128, 512]); RT1 = S.buf("rt1")
                            rt2 = sb(ph, "rt2", [128, 512]); RT2 = S.buf("rt2")
                            v_sb = sb(ph, "v_sb", [128, 20, 128], BF16); VS = S.buf("v_sb")
                            p_sb = [sb(ph, "p_sb%d" % i, [128, 512], BF16) for i in range(2)]
                            PS = [S.buf("p_sb%d" % i) for i in range(2)]
                            pm = [sb(ph, "pm%d" % i, [128, 512], BF16) for i in range(2)]
                            PM = [S.buf("pm%d" % i) for i in range(2)]
                            S.op("pool", lambda: nc.gpsimd.memset(qk[:, :, 0:PAD], 0.0), writes=QK)
                            S.op("pool", lambda: nc.gpsimd.memset(qk[:, :, PAD + S_LEN:], 0.0), writes=QK)
                            NUMB, NUMBb = banks[4], BK[4]
                            DENB, DENBb = banks[5], BK[5]
                            cnt = [0]
                            for hp in range(4):
                                for g, (dil, L) in enumerate(GROUPS):
                                    wt, WTb = nextw()
                                    for t3, off in enumerate((OFF_Q, OFF_K, OFF_V)):
                                        c0 = off + g * 512 + hp * 128
                                        S.op("pool", lambda t3=t3, c0=c0: nc.gpsimd.dma_start(out=wt[:, :, t3, :], in_=wv(l)[:, :, c0:c0 + 128]),
                                             writes=[WTb], dma=WTb)
                                    for t3 in range(2):
                                        for tc in range(4):
                                            bk, BKb = nextbank()
                                            qr, QRb = qraw[tc % 2], QR[tc % 2]
                                            for kc in range(8):
                                                S.op("pe", lambda kc=kc, t3=t3, tc=tc, bk=bk: nc.tensor.matmul(
                                                    bk[:, :], lhsT=wt[:, kc, t3, :], rhs=hT[:, kc, PAD + tc * 512: PAD + (tc + 1) * 512],
                                                    start=(kc == 0), stop=(kc == 7)), reads=[WTb] + HTall, writes=[BKb])
                                            S.op("act", lambda bk=bk, qr=qr: nc.scalar.copy(out=qr[:], in_=bk[:, :]), reads=[BKb], writes=[QRb])
                                            bk2, BK2b = nextbank()
                                            S.op("pe", lambda bk2=bk2, qr=qr: nc.tensor.matmul(bk2[:, :], lhsT=ptb, rhs=qr[:], start=True, stop=True),
                                                 reads=[QRb, CB], writes=[BK2b])
                                            S.op("pool", lambda qr=qr, tc=tc: nc.gpsimd.tensor_tensor(out=rt1[:], in0=qr[:], in1=cosb[:, tc * 512:(tc + 1) * 512],
                                                                                                      op=ALU.mult), reads=[QRb, CB], writes=[RT1])
                                            S.op("dve", lambda bk2=bk2, tc=tc: nc.vector.tensor_tensor(out=rt2[:], in0=bk2[:, :], in1=sinb[:, tc * 512:(tc + 1) * 512],
                                                                                                        op=ALU.mult), reads=[BK2b, CB], writes=[RT2])
                                            S.op("dve", lambda t3=t3, tc=tc: nc.vector.tensor_tensor(out=qk[:, t3, PAD + tc * 512: PAD + (tc + 1) * 512],
                                                                                                      in0=rt1[:], in1=rt2[:], op=ALU.add),
                                                 reads=[RT1, RT2], writes=[QK[t3]])
                                    if g < 2:
                                        ktiles = [(r, m, PAD + r + dil * (128 * m - 64)) for r in range(dil) for m in range(L // 128 + 1)]
                                    else:
                                        ktiles = [(r, 0, PAD + r) for r in range(16)]
                                    for t0 in range(0, len(ktiles), 4):
                                        bk, BKb = nextbank()
                                        grp = ktiles[t0:t0 + 4]
                                        for si, (r, m, st0) in enumerate(grp):
                                            for kc in range(8):
                                                S.op("pe", lambda kc=kc, si=si, st0=st0, bk=bk: nc.tensor.matmul(
                                                    bk[:, si * 128:(si + 1) * 128], lhsT=hT[:, kc, st0: st0 + 127 * dil + 1: dil], rhs=wt[:, kc, 2, :],
                                                    start=(kc == 0), stop=(kc == 7)), reads=[WTb] + HTall, writes=[BKb])
                                        n = len(grp)
                                        S.op("act", lambda bk=bk, t0=t0, n=n: nc.scalar.copy(
                                            out=v_sb[:, t0:t0 + n, :], in_=bk[:, 0:n * 128].rearrange("p (s c) -> p s c", c=128)),
                                            reads=[BKb], writes=[VS])
                                    for hh in range(2):
                                        h0 = hh * 64
                                        for quad in range(4):
                                            if g < 2:
                                                r = 0 if g == 0 else quad
                                                jq = [4 * quad + i for i in range(4)] if g == 0 else [0, 1, 2, 3]
                                                nm = L // 128 + 1
                                                for half in range(2):
                                                    sbk, SBKb = (banks[6], BK[6]) if cnt[0] % 2 == 0 else (banks[3], BK[3])
                                                    pp, PPb = p_sb[cnt[0] % 2], PS[cnt[0] % 2]
                                                    pmm, PMb = pm[cnt[0] % 2], PM[cnt[0] % 2]
                                                    cnt[0] += 1
                                                    for qq in range(2):
                                                        j = jq[2 * half + qq]
                                                        qs = PAD + r + dil * 128 * j
                                                        for ab in range(2):
                                                            ks = PAD + r + dil * (128 * (j + ab) - 64)
                                                            sl = (2 * qq + ab) * 128
                                                            S.op("pe", lambda qs=qs, ks=ks, sl=sl, sbk=sbk: nc.tensor.matmul(
                                                                sbk[:, sl:sl + 128], lhsT=qk[h0:h0 + 64, 1, ks: ks + 127 * dil + 1: dil],
                                                                rhs=qk[h0:h0 + 64, 0, qs: qs + 127 * dil + 1: dil], start=True, stop=True),
                                                                reads=QK, writes=[SBKb])
                                                    first = (j - 1 == 0) if g == 0 else (half == 0)
                                                    last = (jq[2 * half + 1] == L // 128 - 1)
                                                    first = (jq[2 * half] == 0)
                                                    mk = maskb[0] if first else (maskb[2] if last else maskb[1])
                                                    S.op("act", lambda sbk=sbk, pp=pp: nc.scalar.activation(out=pp[:], in_=sbk[:, :], func=AF.Exp, scale=0.125),
                                                         reads=[SBKb], writes=[PPb])
                                                    S.op("pool", lambda pp=pp, pmm=pmm, mk=mk: nc.gpsimd.tensor_tensor(out=pmm[:], in0=pp[:], in1=mk, op=ALU.mult),
                                                         reads=[PPb, CB], writes=[PMb])
                                                    for qq in range(2):
                                                        j = jq[2 * half + qq]
                                                        osl = (2 * half + qq) * 128
                                                        for ab in range(2):
                                                            vt = r * nm + j + ab
                                                            sl = (2 * qq + ab) * 128
                                                            S.op("pe", lambda vt=vt, sl=sl, osl=osl, ab=ab, pmm=pmm: nc.tensor.matmul(
                                                                NUMB[0:64, osl:osl + 128], lhsT=v_sb[:, vt, h0:h0 + 64], rhs=pmm[:, sl:sl + 128],
                                                                start=(ab == 0), stop=(ab == 1), skip_group_check=True), reads=[VS, PMb], writes=[NUMBb])
                                                            S.op("pe", lambda sl=sl, osl=osl, ab=ab, pmm=pmm: nc.tensor.matmul(
                                                                DENB[0:64, osl:osl + 128], lhsT=onesb[:, 0:64], rhs=pmm[:, sl:sl + 128],
                                                                start=(ab == 0), stop=(ab == 1), skip_group_check=True), reads=[CB, PMb], writes=[DENBb])
                                                if g == 0:
                                                    nview = accn[:, hh, quad * 512:(quad + 1) * 512]
                                                    dview = accd[:, hh, quad * 512:(quad + 1) * 512]
                                                else:
                                                    nview = accn[:, hh, quad: S_LEN: 4]
                                                    dview = accd[:, hh, quad: S_LEN: 4]
                                                nsrc, dsrc = NUMB[0:64, :], DENB[0:64, :]
                                            else:
                                                sbk, SBKb = (banks[6], BK[6]) if cnt[0] % 2 == 0 else (banks[3], BK[3])
                                                pp, PPb = p_sb[cnt[0] % 2], PS[cnt[0] % 2]
                                                pmm, PMb = pm[cnt[0] % 2], PM[cnt[0] % 2]
                                                cnt[0] += 1
                                                for k4 in range(4):
                                                    r = 4 * quad + k4
                                                    qs = PAD + r
                                                    S.op("pe", lambda qs=qs, k4=k4, sbk=sbk: nc.tensor.matmul(
                                                        sbk[:, k4 * 128:(k4 + 1) * 128], lhsT=qk[h0:h0 + 64, 1, qs: qs + 2033: 16],
                                                        rhs=qk[h0:h0 + 64, 0, qs: qs + 2033: 16], start=True, stop=True), reads=QK, writes=[SBKb])
                                                S.op("act", lambda sbk=sbk, pp=pp: nc.scalar.activation(out=pp[:], in_=sbk[:, :], func=AF.Exp, scale=0.125),
                                                     reads=[SBKb], writes=[PPb])
                                                S.op("pool", lambda pp=pp, pmm=pmm: nc.gpsimd.tensor_tensor(out=pmm[:], in0=pp[:], in1=maskb[3], op=ALU.mult),
                                                     reads=[PPb, CB], writes=[PMb])
                                                for k4 in range(4):
                                                    r = 4 * quad + k4
                                                    S.op("pe", lambda r=r, k4=k4, pmm=pmm: nc.tensor.matmul(
                                                        NUMB[0:64, k4 * 128:(k4 + 1) * 128], lhsT=v_sb[:, r, h0:h0 + 64], rhs=pmm[:, k4 * 128:(k4 + 1) * 128],
                                                        start=True, stop=True, skip_group_check=True), reads=[VS, PMb], writes=[NUMBb])
                                                    S.op("pe", lambda k4=k4, pmm=pmm: nc.tensor.matmul(
                                                        DENB[0:64, k4 * 128:(k4 + 1) * 128], lhsT=onesb[:, 0:64], rhs=pmm[:, k4 * 128:(k4 + 1) * 128],
                                                        start=True, stop=True, skip_group_check=True), reads=[CB, PMb], writes=[DENBb])
                                                nview = accn[:, hh, :].rearrange("p (c s) -> p s c", s=16)[:, 4 * quad:4 * quad + 4, :]
                                                dview = accd[:, hh, :].rearrange("p (c s) -> p s c", s=16)[:, 4 * quad:4 * quad + 4, :]
                                                nsrc = NUMB[0:64, :].rearrange("p (s c) -> p s c", c=128)
                                                dsrc = DENB[0:64, :].rearrange("p (s c) -> p s c", c=128)
                                            if g == 0:
                                                S.op("act", lambda nview=nview, nsrc=nsrc: nc.scalar.copy(out=nview, in_=nsrc), reads=[NUMBb], writes=[ACCN[hh]])
                                                S.op("dve", lambda dview=dview, dsrc=dsrc: nc.vector.tensor_copy(out=dview, in_=dsrc), reads=[DENBb], writes=[ACCD[hh]])
                                            else:
                                                S.op("dve", lambda nview=nview, nsrc=nsrc: nc.vector.tensor_tensor(out=nview, in0=nview, in1=nsrc, op=ALU.add),
                                                     reads=[NUMBb], writes=[ACCN[hh]])
                                                S.op("dve", lambda dview=dview, dsrc=dsrc: nc.vector.tensor_tensor(out=dview, in0=dview, in1=dsrc, op=ALU.add),
                                                     reads=[DENBb], writes=[ACCD[hh]])
                                for hh in range(2):
                                    S.op("dve", lambda hh=hh: nc.vector.reciprocal(out=accd[:, hh, :], in_=accd[:, hh, :]), writes=[ACCD[hh]])
                                    S.op("dve", lambda hh=hh, hp=hp: nc.vector.tensor_tensor(out=oT[hh * 64:(hh + 1) * 64, hp, :], in0=accn[:, hh, :],
                                                                                             in1=accd[:, hh, :], op=ALU.mult),
                                         reads=[ACCN[hh], ACCD[hh]], writes=[OT[hp]])
                        S.barrier()
                        with ExitStack() as ph:
                            mT = sb(ph, "mT", [128, 8, 1024], BF16); MT = S.buf("mT")
                            sg = [sb(ph, "sg%d" % i, [128, 512]) for i in range(2)]; SG = [S.buf("sg%d" % i) for i in range(2)]
                            m1 = sb(ph, "m1", [128, 512]); M1 = S.buf("m1")
                            m2 = sb(ph, "m2", [128, 512]); M2 = S.buf("m2")
                            x1 = [sb(ph, "x1_%d" % i, [128, D]) for i in range(2)]; X1 = [S.buf("x1_%d" % i) for i in range(2)]
                            rec = [sb(ph, "rec%d" % i, [128, RECW]) for i in range(2)]; REC = [S.buf("rec%d" % i) for i in range(2)]
                            x1T = sb(ph, "x1T", [128, 8, 128]); X1T = S.buf("x1T")
                            lg = sb(ph, "lg", [128, NE]); LG = S.buf("lg")
                            for hs in range(2):
                                for dc in range(8):
                                    wt, WTb = nextw()
                                    for t3 in range(2):
                                        c0 = OFF_G + t3 * 1024 + dc * 128
                                        S.op("pool", lambda t3=t3, c0=c0: nc.gpsimd.dma_start(out=wt[:, :, t3, :], in_=wv(l)[:, :, c0:c0 + 128]),
                                             writes=[WTb], dma=WTb)
                                    S.op("pool", lambda dc=dc: nc.gpsimd.dma_start(
                                        out=wt[:, 0:4, 2, :], in_=w_conv_out[l].rearrange("(kc k) c -> k kc c", k=128)[:, :, dc * 128:(dc + 1) * 128]),
                                        writes=[WTb], dma=WTb)
                                    S.op("pool", lambda dc=dc: nc.gpsimd.dma_start(
                                        out=wt[:, 4:8, 2, :], in_=w_attn_out[l].rearrange("(kc k) c -> k kc c", k=128)[:, :, dc * 128:(dc + 1) * 128]),
                                        writes=[WTb], dma=WTb)
                                    for tcl in range(2):
                                        tok0 = hs * 1024 + tcl * 512
                                        gb = []
                                        for t3 in range(2):
                                            bk, BKb = nextbank()
                                            gb.append((bk, BKb))
                                            for kc in range(8):
                                                S.op("pe", lambda kc=kc, t3=t3, bk=bk: nc.tensor.matmul(
                                                    bk[:, :], lhsT=wt[:, kc, t3, :], rhs=hT[:, kc, PAD + tok0: PAD + tok0 + 512],
                                                    start=(kc == 0), stop=(kc == 7)), reads=[WTb] + HTall, writes=[BKb])
                                            S.op("act", lambda t3=t3, bk=bk: nc.scalar.activation(out=sg[t3][:], in_=bk[:, :], func=AF.Sigmoid),
                                                 reads=[BKb], writes=[SG[t3]])
                                        bkc, BKc = nextbank()
                                        for kc in range(4):
                                            S.op("pe", lambda kc=kc, bkc=bkc: nc.tensor.matmul(
                                                bkc[:, :], lhsT=wt[:, kc, 2, :], rhs=ucT[:, kc, tok0:tok0 + 512], start=(kc == 0), stop=(kc == 3)),
                                                reads=[WTb] + UCT, writes=[BKc])
                                        bka, BKa = nextbank()
                                        for kc in range(4):
                                            S.op("pe", lambda kc=kc, bka=bka: nc.tensor.matmul(
                                                bka[:, :], lhsT=wt[:, 4 + kc, 2, :], rhs=oT[:, kc, tok0:tok0 + 512], start=(kc == 0), stop=(kc == 3)),
                                                reads=[WTb] + OT, writes=[BKa])
                                        S.op("dve", lambda bkc=bkc: nc.vector.tensor_tensor(out=m1[:], in0=sg[0][:], in1=bkc[:, :], op=ALU.mult),
                                             reads=[SG[0], BKc], writes=[M1])
                                        S.op("dve", lambda bka=bka: nc.vector.tensor_tensor(out=m2[:], in0=sg[1][:], in1=bka[:, :], op=ALU.mult),
                                             reads=[SG[1], BKa], writes=[M2])
                                        S.op("dve", lambda dc=dc, tcl=tcl: nc.vector.tensor_tensor(out=mT[:, dc, tcl * 512:(tcl + 1) * 512], in0=m1[:], in1=m2[:],
                                                                                                  op=ALU.add), reads=[M1, M2], writes=[MT])
                                for tt in range(8):
                                    tok = hs * 1024 + tt * 128
                                    gt = s * 16 + hs * 8 + tt
                                    xt, XTb = xts[tt % 2], XT[tt % 2]
                                    xx, XXb = x1[tt % 2], X1[tt % 2]
                                    rc, RCb = rec[tt % 2], REC[tt % 2]
                                    ss, rs, mx, se = small[:, 2:3], small[:, 3:4], small[:, 4:5], small[:, 5:6]
                                    S.op("sp", lambda: nc.sync.dma_start(out=xt[:], in_=xsrc[r0 + tok: r0 + tok + 128, :]),
                                         reads=[XSRC], writes=[XTb], dma=XTb)
                                    for half in range(2):
                                        bk, BKb = nextbank()
                                        for kc in range(8):
                                            S.op("pe", lambda kc=kc, half=half, bk=bk: nc.tensor.matmul(
                                                bk[:, :], lhsT=mT[:, kc, tt * 128:(tt + 1) * 128], rhs=wo[:, kc, half * 512:(half + 1) * 512],
                                                start=(kc == 0), stop=(kc == 7)), reads=[MT, WO], writes=[BKb])
                                        S.op("dve", lambda half=half, bk=bk: nc.vector.tensor_tensor(
                                            out=xx[:, half * 512:(half + 1) * 512], in0=xt[:, half * 512:(half + 1) * 512], in1=bk[:, :], op=ALU.add),
                                            reads=[XTb, BKb], writes=[XXb])
                                    S.op("sp", lambda: nc.sync.dma_start(out=xbuf[r0 + tok: r0 + tok + 128, :], in_=xx[:]),
                                         reads=[XXb], writes=[XB[s]], dma=XXb)
                                    S.op("act", lambda: nc.scalar.activation(out=junk[:], in_=xx[:], func=AF.Square, accum_out=ss),
                                         reads=[XXb], writes=[JUNK, SM[2]])
                                    rstd_ops(ss, rs, SM[2], SM[3])
                                    for q4 in range(2):
                                        bk, BKb = nextbank()
                                        for k4 in range(4):
                                            kc = q4 * 4 + k4
                                            S.op("pe", lambda kc=kc, k4=k4, bk=bk: nc.tensor.transpose(
                                                out=bk[:, k4 * 128:(k4 + 1) * 128], in_=xx[:, kc * 128:(kc + 1) * 128], identity=identf),
                                                reads=[XXb, CSTF], writes=[BKb])
                                        S.op("act", lambda q4=q4, bk=bk: nc.scalar.copy(out=x1T[:, q4 * 4:(q4 + 1) * 4, :],
                                                                                        in_=bk[:, :].rearrange("p (k t) -> p k t", k=4)),
                                             reads=[BKb], writes=[X1T])
                                    bk, BKb = nextbank()
                                    for kc in range(8):
                                        S.op("pe", lambda kc=kc, bk=bk: nc.tensor.matmul(bk[:, 0:NE], lhsT=x1T[:, kc, :], rhs=wrg[:, kc, :],
                                                                                         start=(kc == 0), stop=(kc == 7)), reads=[X1T, WRG], writes=[BKb])
                                    S.op("dve", lambda bk=bk: nc.vector.tensor_scalar(out=lg[:], in0=bk[:, 0:NE], scalar1=rs, scalar2=None, op0=ALU.mult),
                                         reads=[BKb, SM[3]], writes=[LG])
                                    S.op("dve", lambda: nc.vector.reduce_max(out=mx, in_=lg[:], axis=AX.X), reads=[LG], writes=[SM[4]])
                                    S.op("dve", lambda: nc.vector.tensor_scalar(out=mx, in0=mx, scalar1=-1.0, scalar2=None, op0=ALU.mult),
                                         reads=[SM[4]], writes=[SM[4]])
                                    S.op("act", lambda: nc.scalar.activation(out=lg[:], in_=lg[:], func=AF.Exp, bias=mx, accum_out=se),
                                         reads=[SM[4]], writes=[LG, SM[5]])
                                    S.op("dve", lambda: nc.vector.reciprocal(out=se, in_=se), writes=[SM[5]])
                                    S.op("dve", lambda gt=gt: nc.vector.tensor_scalar(out=aff_all[:, gt, :], in0=lg[:], scalar1=se, scalar2=None, op0=ALU.mult),
                                         reads=[LG, SM[5]], writes=[AFF])
                                    S.op("dve", lambda: nc.vector.scalar_tensor_tensor(out=rc[:, 0:D], in0=xx[:], scalar=rs, in1=g2bc[:],
                                                                                        op0=ALU.mult, op1=ALU.mult),
                                         reads=[XXb, SM[3], G2BC], writes=[RCb])
                                    S.op("act", lambda gt=gt: nc.scalar.copy(out=rc[:, D:D + NE], in_=aff_all[:, gt, :]), reads=[AFF], writes=[RCb])
                                    S.op("dve", lambda gt=gt: nc.vector.tensor_scalar(out=rc[:, D + NE:D + NE + 1], in0=iota, scalar1=float(gt * 128),
                                                                                       scalar2=None, op0=ALU.add), reads=[CSTF], writes=[RCb])
                                    S.op("sp", lambda gt=gt: nc.sync.dma_start(out=h2rec[gt * 128:(gt + 1) * 128, :], in_=rc[:]),
                                         reads=[RCb], writes=[H2R], dma=RCb)
                        S.barrier()
                        S.release_since(mk_seq)
                S.barrier()
                S.release_since(mk_layer)
                if dbg == "A":
                    DBG = S.buf("dbg")
                    for q0 in range(0, NT, 2048):
                        S.op("sp", lambda q0=q0: nc.sync.dma_start(out=y_out[q0:q0 + 2048, :], in_=xbuf[q0:q0 + 2048, :]), writes=[DBG], dma=DBG)
                    S.barrier()
                    return nc
                if dbg is not None and l == 0:
                    DBG = S.buf("dbg")
                    for q0 in range(0, NT, 2048):
                        S.op("sp", lambda q0=q0: nc.sync.dma_start(out=y1_out[q0:q0 + 2048, :], in_=xbuf[q0:q0 + 2048, :]), writes=[DBG], dma=DBG)
                    S.barrier()
                XSD = S.buf("xs_d")
                XBALL = S.buf("xball")
                mk_b = S.mark()
                with ExitStack() as lb:
                    G = sb(lb, "G", [128, NT]); GB = S.buf("G")
                    stg = sb(lb, "stg", [NE, 512]); STG = S.buf("stg")
                    cmpj = sb(lb, "cmpj", [128, max(nP, nS) * S_LEN], BF16); CMPJ = S.buf("cmpj")
                    bis = sb(lb, "bis", [128, 16]); BIS = S.buf("bis")
                    thb = sb(lb, "thb", [NE, 2, 128]); THB = S.buf("thb")
                    thr = sb(lb, "thr", [128, 2, NE]); THR = S.buf("thr")
                    Mk = sb(lb, "Mk", [128, NTILE, NE]); MK = S.buf("Mk")
                    Mkb = sb(lb, "Mkb", [128, NTILE * NE], BF16); MKB = S.buf("Mkb")
                    pre = sb(lb, "pre", [128, NTILE, NE]); PRE = S.buf("pre")
                    tot = sb(lb, "tot", [128, NTILE, NE]); TOT = S.buf("tot")
                    cum = sb(lb, "cum", [128, NTILE, NE]); CUM = S.buf("cum")
                    onesf = sb(lb, "onesf", [128, 128]); ONF = S.buf("onesf")
                    hs2 = sb(lb, "hs2", [128, NTILE, NE]); HS2 = S.buf("hs2")
                    cum2 = sb(lb, "cum2", [128, NTILE, NE]); CUM2 = S.buf("cum2")
                    idxi = sb(lb, "idxi", [128, NTILE, NE], I32); IDX = S.buf("idxi")
                    recl = [sb(lb, "recl%d" % i, [128, RECW]) for i in range(3)]; RECL = [S.buf("recl%d" % i) for i in range(3)]
                    AFT = S.buf("affT_d"); GTH = S.buf("gath_d")
                    for e in range(NE):
                        S.op("sp", lambda e=e: nc.sync.dma_start(out=xs_d[e][:, :], in_=xs_init[:, :]), writes=[XSD], dma=XSD)
                    for t0 in range(0, NTILE, 4):
                        bk, BKb = nextbank()
                        for k4 in range(4):
                            S.op("pe", lambda k4=k4, t0=t0, bk=bk: nc.tensor.transpose(out=bk[0:NE, k4 * 128:(k4 + 1) * 128],
                                                                                       in_=aff_all[:, t0 + k4, :], identity=identf),
                                 reads=[AFF, CSTF], writes=[BKb])
                        S.op("act", lambda bk=bk: nc.scalar.copy(out=stg[:], in_=bk[0:NE, :]), reads=[BKb], writes=[STG])
                        S.op("sp", lambda t0=t0: nc.sync.dma_start(out=affT_d[:, t0 * 128:(t0 + 4) * 128], in_=stg[:]),
                             reads=[STG], writes=[AFT], dma=STG)
                    S.barrier()
                    if stop == "B1":
                        return nc
                    S.op("pool", lambda: nc.gpsimd.collective_compute("AllGather", ALU.bypass, replica_groups=[list(range(NCORE))],
                                                                       ins=[affT_d[:, :]], outs=[gath_d[:, :]]),
                         reads=[AFT], writes=[GTH], dma=GTH, inc=1)
                    S.op("sp", lambda: nc.sync.dma_start(out=G[:], in_=gath_d[:, :]), reads=[GTH], writes=[GB], dma=GB)
                    if stop == "B2":
                        S.barrier()
                        return nc
                    lo, hi, mid, cntv, ge, tmp = bis[:, 0:2], bis[:, 2:4], bis[:, 4:6], bis[:, 6:8], bis[:, 8:10], bis[:, 10:12]
                    S.op("dve", lambda: nc.vector.memset(lo, 0.0), writes=[BIS])
                    S.op("dve", lambda: nc.vector.memset(hi, 1.0001), writes=[BIS])
                    grp_cols = [(0, nP * S_LEN), (nP * S_LEN, NT)]
                    for it in range(34):
                        S.op("dve", lambda: nc.vector.tensor_tensor(out=mid, in0=lo, in1=hi, op=ALU.add), writes=[BIS])
                        S.op("dve", lambda: nc.vector.tensor_scalar(out=mid, in0=mid, scalar1=0.5, scalar2=None, op0=ALU.mult), writes=[BIS])
                        for gi, (c0, c1) in enumerate(grp_cols):
                            S.op("dve", lambda gi=gi, c0=c0, c1=c1: nc.vector.tensor_scalar(
                                out=cmpj[:, 0:c1 - c0], in0=G[:, c0:c1], scalar1=bis[:, 4 + gi:5 + gi], scalar2=0.0,
                                op0=ALU.is_ge, op1=ALU.add, accum_out=bis[:, 6 + gi:7 + gi]), reads=[GB], writes=[CMPJ, BIS])
                        bk, BKb = nextbank()
                        S.op("pe", lambda bk=bk: nc.tensor.matmul(bk[:, 0:2], lhsT=gsel, rhs=cntv, start=True, stop=True),
                             reads=[BIS, CSTF], writes=[BKb])
                        S.op("dve", lambda bk=bk: nc.vector.tensor_tensor(out=ge, in0=bk[:, 0:2], in1=capv[:], op=ALU.is_ge),
                             reads=[BKb, CAPV], writes=[BIS])
                        S.op("dve", lambda: nc.vector.tensor_tensor(out=tmp, in0=mid, in1=lo, op=ALU.subtract), writes=[BIS])
                        S.op("dve", lambda: nc.vector.tensor_tensor(out=tmp, in0=tmp, in1=ge, op=ALU.mult), writes=[BIS])
                        S.op("dve", lambda: nc.vector.tensor_tensor(out=lo, in0=lo, in1=tmp, op=ALU.add), writes=[BIS])
                        S.op("dve", lambda: nc.vector.tensor_tensor(out=tmp, in0=hi, in1=mid, op=ALU.subtract), writes=[BIS])
                        S.op("dve", lambda: nc.vector.tensor_tensor(out=tmp, in0=tmp, in1=ge, op=ALU.mult), writes=[BIS])
                        S.op("dve", lambda: nc.vector.tensor_tensor(out=hi, in0=mid, in1=tmp, op=ALU.add), writes=[BIS])
                    if dbg == "Bonly":
                        print("nops at B3", S.nops)
                    if stop == "B3":
                        S.barrier()
                        return nc
                    S.op("dve", lambda: nc.vector.memset(thb[:], 1.0), writes=[THB])
                    bk, BKb = nextbank()
                    for gi in range(2):
                        S.op("dve", lambda gi=gi: nc.vector.tensor_scalar(out=thb[:, gi, :], in0=thb[:, gi, :], scalar1=bis[0:NE, gi:gi + 1],
                                                                           scalar2=None, op0=ALU.mult), reads=[BIS], writes=[THB])
                        S.op("pe", lambda gi=gi, bk=bk: nc.tensor.matmul(bk[:, gi * NE:(gi + 1) * NE], lhsT=thb[:, gi, :], rhs=i16, start=True, stop=True),
                             reads=[THB, CSTF], writes=[BKb])
                    S.op("act", lambda bk=bk: nc.scalar.copy(out=thr[:], in_=bk[:, 0:2 * NE].rearrange("p (g e) -> p g e", g=2)), reads=[BKb], writes=[THR])
                    for gt in range(NTILE):
                        gi = 0 if gt < nPT else 1
                        S.op("dve", lambda gt=gt, gi=gi: nc.vector.tensor_tensor(out=Mk[:, gt, :], in0=aff_all[:, gt, :], in1=thr[:, gi, :], op=ALU.is_ge),
                             reads=[AFF, THR], writes=[MK])
                    Mkf = Mk[:, :, :].rearrange("p t e -> p (t e)")
                    S.op("dve", lambda: nc.vector.tensor_copy(out=Mkb[:], in_=Mkf), reads=[MK], writes=[MKB])
                    ncol = NTILE * NE
                    for c0 in range(0, ncol, 512):
                        n = min(512, ncol - c0)
                        for lhs, dst, DST in ((Ub, pre, PRE), (onesb, tot, TOT)):
                            bk, BKb = nextbank()
                            S.op("pe", lambda bk=bk, lhs=lhs, c0=c0, n=n: nc.tensor.matmul(bk[:, 0:n], lhsT=lhs, rhs=Mkb[:, c0:c0 + n], start=True, stop=True),
                                 reads=[MKB, CB], writes=[BKb])
                            S.op("act", lambda bk=bk, dst=dst, c0=c0, n=n: nc.scalar.copy(
                                out=dst[:, :, :].rearrange("p t e -> p (t e)")[:, c0:c0 + n], in_=bk[:, 0:n]), reads=[BKb], writes=[DST])
                    hsA, hsB = cum, hs2
                    S.op("dve", lambda: nc.vector.tensor_copy(out=hsA[:], in_=Mk[:]), reads=[MK], writes=[CUM])
                    HSB = {id(cum): CUM, id(hs2): HS2}
                    sft = 1
                    while sft < NTILE:
                        S.op("dve", lambda hsA=hsA, hsB=hsB, sft=sft: nc.vector.tensor_tensor(out=hsB[:, sft:, :], in0=hsA[:, sft:, :], in1=hsA[:, :NTILE - sft, :], op=ALU.add),
                             reads=[HSB[id(hsA)]], writes=[HSB[id(hsB)]])
                        S.op("dve", lambda hsA=hsA, hsB=hsB, sft=sft: nc.vector.tensor_copy(out=hsB[:, :sft, :], in_=hsA[:, :sft, :]),
                             reads=[HSB[id(hsA)]], writes=[HSB[id(hsB)]])
                        hsA, hsB = hsB, hsA
                        sft *= 2
                    S.op("dve", lambda: nc.vector.memset(onesf[:], 1.0), writes=[ONF])
                    hf = hsA[:, :, :].rearrange("p t e -> p (t e)")
                    cumf = cum2[:, :, :].rearrange("p t e -> p (t e)")
                    for c0 in range(0, ncol, 512):
                        n = min(512, ncol - c0)
                        bk, BKb = nextbank()
                        S.op("pe", lambda bk=bk, c0=c0, n=n: nc.tensor.matmul(bk[:, 0:n], lhsT=onesf[:], rhs=hf[:, c0:c0 + n], start=True, stop=True),
                             reads=[HSB[id(hsA)], ONF], writes=[BKb])
                        S.op("act", lambda bk=bk, c0=c0, n=n: nc.scalar.copy(out=cumf[:, c0:c0 + n], in_=bk[:, 0:n]), reads=[BKb], writes=[CUM2])
                    pf = pre[:, :, :].rearrange("p t e -> p (t e)")
                    S.op("dve", lambda: nc.vector.tensor_tensor(out=pf, in0=pf, in1=cum2[:, :, :].rearrange("p t e -> p (t e)"), op=ALU.add),
                         reads=[CUM2], writes=[PRE])
                    S.op("dve", lambda: nc.vector.tensor_tensor(out=pf, in0=pf, in1=tot[:, :, :].rearrange("p t e -> p (t e)"), op=ALU.subtract),
                         reads=[TOT], writes=[PRE])
                    S.op("dve", lambda: nc.vector.tensor_scalar(out=Mkf, in0=Mkf, scalar1=-1.0e6, scalar2=1.0e6, op0=ALU.mult, op1=ALU.add), writes=[MK])
                    S.op("dve", lambda: nc.vector.tensor_tensor(out=pf, in0=pf, in1=Mkf, op=ALU.add), reads=[MK], writes=[PRE])
                    S.op("dve", lambda: nc.vector.tensor_copy(out=idxi[:, :, :].rearrange("p t e -> p (t e)"), in_=pf), reads=[PRE], writes=[IDX])
                    if dbg == "Bonly":
                        print("nops at B4", S.nops)
                    if stop == "B4":
                        S.barrier()
                        return nc
                    for gt in range(NTILE):
                        rl, RLb = recl[gt % 3], RECL[gt % 3]
                        S.op("sp", lambda gt=gt, rl=rl: nc.sync.dma_start(out=rl[:], in_=h2rec[gt * 128:(gt + 1) * 128, :]),
                             reads=[H2R], writes=[RLb], dma=RLb)
                        for e in range(NE):
                            S.op("pool", lambda gt=gt, e=e, rl=rl: nc.gpsimd.indirect_dma_start(
                                out=xs_d[e][:, :], out_offset=bass.IndirectOffsetOnAxis(ap=idxi[:, gt, e:e + 1], axis=0),
                                in_=rl[:], in_offset=None, bounds_check=bc_slots, oob_is_err=False),
                                reads=[RLb, IDX, XSD], dma=RLb)
                    if dbg is not None and l == 0:
                        S.op("sp", lambda: nc.sync.dma_start(out=dbg_out[:, 0:16], in_=bis[:]), reads=[BIS], writes=[DBG], dma=DBG)
                        S.op("sp", lambda: nc.sync.dma_start(out=dbg_out[:, 16:48], in_=thr[:, :, :].rearrange("p g e -> p (g e)")), reads=[THR], writes=[DBG], dma=DBG)
                        S.op("sp", lambda: nc.sync.dma_start(out=dbg_out[:, 48:64], in_=tot[:, NTILE - 1, :]), reads=[TOT], writes=[DBG], dma=DBG)
                    S.barrier()
                    S.release_since(mk_b)
                if dbg == "Bonly":
                    for k, e in enumerate((0, 5)):
                        S.op("sp", lambda k=k, e=e: nc.sync.dma_start(out=y_out[k * cap_slots:(k + 1) * cap_slots, :], in_=xs_d[e][:, 0:D]), writes=[DBG], dma=DBG)
                        S.op("sp", lambda k=k, e=e: nc.sync.dma_start(out=y1_out[k * cap_slots:(k + 1) * cap_slots, 0:32], in_=xs_d[e][:, D:D + 32]), writes=[DBG], dma=DBG)
                    S.barrier()
                    return nc
                if dbg == "B":
                    return nc
                mk_c = S.mark()
                with ExitStack() as lm:
                    xsfs = [sb(lm, "xsf%d" % i, [128, RECW]) for i in range(2)]; XSFS = [S.buf("xsf%d" % i) for i in range(2)]
                    xsbs = [sb(lm, "xsb%d" % i, [128, D], BF16) for i in range(2)]; XSBS = [S.buf("xsb%d" % i) for i in range(2)]
                    xsT = sb(lm, "xsT", [128, 8, cap_slots], BF16); XST = S.buf("xsT")
                    gT = sb(lm, "gT", [128, 16, cap_slots], BF16); GT = S.buf("gT")
                    gate_all = sb(lm, "gate_all", [128, CAPT]); GA = S.buf("gate_all")
                    tok_i = sb(lm, "tok_i", [128, CAPT], I32); TK = S.buf("tok_i")
                    wgu = [sb(lm, "wgu%d" % i, [128, 8, 2, 512], BF16) for i in range(2)]; WGU = [S.buf("wgu%d" % i) for i in range(2)]
                    wd = sb(lm, "wd", [128, 16, D], BF16); WD = S.buf("wd")
                    sil = [sb(lm, "sil%d" % i, [128, 512]) for i in range(2)]; SIL = [S.buf("sil%d" % i) for i in range(2)]
                    y_sbs = [sb(lm, "y_sb%d" % i, [128, D]) for i in range(2)]; YSS = [S.buf("y_sb%d" % i) for i in range(2)]
                    sgs = [(a, min(512, cap_slots - a)) for a in range(0, cap_slots, 512)]
                    wq = [0]
                    for e in range(NE):
                        for stile in range(CAPT):
                            xsf, XSF = xsfs[stile % 2], XSFS[stile % 2]
                            xsb, XSBb = xsbs[stile % 2], XSBS[stile % 2]
                            S.op("sp", lambda e=e, stile=stile, xsf=xsf: nc.sync.dma_start(out=xsf[:], in_=xs_d[e][stile * 128:(stile + 1) * 128, :]),
                                 reads=[XSD], writes=[XSF], dma=XSF)
                            S.op("act", lambda xsf=xsf, xsb=xsb: nc.scalar.copy(out=xsb[:], in_=xsf[:, 0:D]), reads=[XSF], writes=[XSBb])
                            S.op("dve", lambda e=e, stile=stile, xsf=xsf: nc.vector.tensor_copy(out=gate_all[:, stile:stile + 1], in_=xsf[:, D + e:D + e + 1]),
                                 reads=[XSF], writes=[GA])
                            S.op("dve", lambda stile=stile, xsf=xsf: nc.vector.tensor_copy(out=tok_i[:, stile:stile + 1], in_=xsf[:, D + NE:D + NE + 1]),
                                 reads=[XSF], writes=[TK])
                            for kc in range(8):
                                S.op("pe", lambda kc=kc, xsb=xsb: nc.tensor.transpose(out=bankb[:, kc * 128:(kc + 1) * 128], in_=xsb[:, kc * 128:(kc + 1) * 128],
                                                                             identity=identb), reads=[XSBb, CB], writes=[BKB])
                            S.op("act", lambda stile=stile: nc.scalar.copy(out=xsT[:, :, stile * 128:(stile + 1) * 128],
                                                                           in_=bankb[:, :].rearrange("p (k t) -> p k t", k=8)), reads=[BKB], writes=[XST])
                        for fq in range(4):
                            wg, WGb = wgu[wq[0] % 2], WGU[wq[0] % 2]
                            wq[0] += 1
                            for t2, wsrc in enumerate((w_gate, w_up)):
                                S.op("pool", lambda t2=t2, wsrc=wsrc, wg=wg, e=e, fq=fq: nc.gpsimd.dma_start(
                                    out=wg[:, :, t2, :], in_=wsrc[l, e].rearrange("(kc k) f -> k kc f", k=128)[:, :, fq * 512:(fq + 1) * 512]),
                                    writes=[WGb], dma=WGb)
                            for fc4 in range(4):
                                fc = fq * 4 + fc4
                                for si, (a0, an) in enumerate(sgs):
                                    bka, BKa = nextbank()
                                    bku, BKu = nextbank()
                                    for t2, bk in ((0, bka), (1, bku)):
                                        for kc in range(8):
                                            S.op("pe", lambda kc=kc, t2=t2, bk=bk, wg=wg, fc4=fc4, a0=a0, an=an: nc.tensor.matmul(
                                                bk[:, 0:an], lhsT=wg[:, kc, t2, fc4 * 128:(fc4 + 1) * 128], rhs=xsT[:, kc, a0:a0 + an],
                                                start=(kc == 0), stop=(kc == 7)), reads=[WGb, XST], writes=[BKa if t2 == 0 else BKu])
                                    sl, SLb = sil[si % 2], SIL[si % 2]
                                    S.op("act", lambda bka=bka, sl=sl, an=an: nc.scalar.activation(out=sl[:, 0:an], in_=bka[:, 0:an], func=AF.Silu),
                                         reads=[BKa], writes=[SLb])
                                    S.op("dve", lambda bku=bku, sl=sl, fc=fc, a0=a0, an=an: nc.vector.tensor_tensor(
                                        out=gT[:, fc, a0:a0 + an], in0=sl[:, 0:an], in1=bku[:, 0:an], op=ALU.mult), reads=[SLb, BKu], writes=[GT])
                        S.op("pool", lambda e=e: nc.gpsimd.dma_start(out=wd[:, 0:8, :], in_=w_down[l, e].rearrange("(fc f) d -> f fc d", f=128)[:, 0:8, :]),
                             writes=[WD], dma=WD)
                        S.op("pool", lambda e=e: nc.gpsimd.dma_start(out=wd[:, 8:16, :], in_=w_down[l, e].rearrange("(fc f) d -> f fc d", f=128)[:, 8:16, :]),
                             writes=[WD], dma=WD)
                        for stile in range(CAPT):
                            y_sb, YS = y_sbs[stile % 2], YSS[stile % 2]
                            for dh in range(2):
                                bk, BKb = nextbank()
                                for fc in range(16):
                                    S.op("pe", lambda fc=fc, bk=bk, stile=stile, dh=dh: nc.tensor.matmul(
                                        bk[:, :], lhsT=gT[:, fc, stile * 128:(stile + 1) * 128], rhs=wd[:, fc, dh * 512:(dh + 1) * 512],
                                        start=(fc == 0), stop=(fc == 15)), reads=[GT, WD], writes=[BKb])
                                S.op("dve", lambda bk=bk, stile=stile, dh=dh, y_sb=y_sb: nc.vector.tensor_scalar(
                                    out=y_sb[:, dh * 512:(dh + 1) * 512], in0=bk[:, :], scalar1=gate_all[:, stile:stile + 1], scalar2=None, op0=ALU.mult),
                                    reads=[BKb, GA], writes=[YS])
                            S.op("pool", lambda stile=stile, y_sb=y_sb: nc.gpsimd.indirect_dma_start(
                                out=xbuf[:, :], out_offset=bass.IndirectOffsetOnAxis(ap=tok_i[:, stile:stile + 1], axis=0),
                                in_=y_sb[:], in_offset=None, bounds_check=bc_rows, oob_is_err=True, compute_op=ALU.add),
                                reads=[YS, TK], writes=[XBALL], dma=YS)
                    S.barrier()
                    S.release_since(mk_c)
                if dbg == "C":
                    for q0 in range(0, NT, 2048):
                        S.op("sp", lambda q0=q0: nc.sync.dma_start(out=y_out[q0:q0 + 2048, :], in_=xbuf[q0:q0 + 2048, :]), writes=[DBG], dma=DBG)
                    S.barrier()
                    return nc
            with ExitStack() as lf:
                gf = sb(lf, "gf", [128, D]); GF = S.buf("gf")
                xf = [sb(lf, "xf%d" % i, [128, D]) for i in range(2)]; XF = [S.buf("xf%d" % i) for i in range(2)]
                yf = [sb(lf, "yf%d" % i, [128, D]) for i in range(2)]; YF = [S.buf("yf%d" % i) for i in range(2)]
                junk2 = sb(lf, "junk2", [128, D], BF16); J2 = S.buf("junk2")
                YO = S.buf("yout")
                S.op("sp", lambda: nc.sync.dma_start(out=gf[:], in_=final_g[0:1, :].to_broadcast([128, D])), writes=[GF], dma=GF)
                for gt in range(NTILE):
                    xx, XXb = xf[gt % 2], XF[gt % 2]
                    yy, YYb = yf[gt % 2], YF[gt % 2]
                    ss, rs = small[:, 8:9], small[:, 9:10]
                    S.op("sp", lambda gt=gt, xx=xx: nc.sync.dma_start(out=xx[:], in_=xbuf[gt * 128:(gt + 1) * 128, :]), writes=[XXb], dma=XXb)
                    S.op("act", lambda xx=xx: nc.scalar.activation(out=junk2[:], in_=xx[:], func=AF.Square, accum_out=ss),
                         reads=[XXb], writes=[J2, SM[8]])
                    rstd_ops(ss, rs, SM[8], SM[9])
                    S.op("dve", lambda xx=xx, yy=yy: nc.vector.scalar_tensor_tensor(out=yy[:], in0=xx[:], scalar=rs, in1=gf[:], op0=ALU.mult, op1=ALU.mult),
                         reads=[XXb, SM[9], GF], writes=[YYb])
                    S.op("sp", lambda gt=gt, yy=yy: nc.sync.dma_start(out=y_out[gt * 128:(gt + 1) * 128, :], in_=yy[:]), reads=[YYb], writes=[YO], dma=YYb)
            S.barrier()

      except StopBuild:
          S.stop_at = None
          S.barrier()
    return nc


def run(inputs, nP, nS, cap_slots, dbg=None):
    nc = build(nP, nS, cap_slots, dbg)
    cstf, cstb = make_consts()
    NT = (nP + nS) * S_LEN
    xs_init = np.zeros((cap_slots, RECW), np.float32)
    xs_init[:, D + NE] = NT + (np.arange(cap_slots) % 128)
    capv = np.zeros((128, 2), np.float32)
    capv[:, 0] = 2 * (nP * NCORE * S_LEN) // NE
    capv[:, 1] = 2 * (nS * NCORE * S_LEN) // NE
    f = lambda a: np.ascontiguousarray(np.asarray(a, dtype=np.float32))
    shared = {k: f(inputs[k]) for k in ("norm1_g", "w_in", "conv_w", "w_conv_out", "w_attn_out", "w_o", "norm2_g",
                                        "w_router", "w_gate", "w_up", "w_down")}
    shared["final_g"] = f(inputs["final_g"]).reshape(1, D)
    shared.update(cstf=cstf, cstb=cstb, xs_init=xs_init, capv=capv)
    xp = f(inputs["x_prompt"])
    xs = f(inputs["x_sample"])
    in_maps = []
    for c in range(NCORE):
        xi = np.concatenate([xp[c * nP:(c + 1) * nP].reshape(-1, D), xs[c * nS:(c + 1) * nS].reshape(-1, D)], 0)
        m = dict(shared)
        m["x_in"] = np.ascontiguousarray(xi)
        in_maps.append(m)
    res = run_bass_kernel_spmd(nc, in_maps, core_ids=list(range(NCORE)))
    if dbg is not None:
        return res
    yp = np.zeros_like(xp)
    ys = np.zeros_like(xs)
    for c in range(NCORE):
        y = res.results[c]["y"]
        yp[c * nP:(c + 1) * nP] = y[:nP * S_LEN].reshape(nP, S_LEN, D)
        ys[c * nS:(c + 1) * nS] = y[nP * S_LEN:].reshape(nS, S_LEN, D)
    return yp, ys


def kernel(**inputs):
    yp, ys = run(inputs, 4, 2, 1792)
    return (yp, ys)
```

```python
import numpy as np
from contextlib import ExitStack
import concourse.bass as bass
import concourse.mybir as mybir
from concourse.bass_utils import run_bass_kernel_spmd

F32 = mybir.dt.float32
BF16 = mybir.dt.bfloat16
I32 = mybir.dt.int32
ALU = mybir.AluOpType
AF = mybir.ActivationFunctionType
AX = mybir.AxisListType

NCORE = 8
D = 1024
S_LEN = 2048
DEPTH = 2
D_IN = 8192
OFF_Q, OFF_K, OFF_V, OFF_G = 1536, 3072, 4608, 6144
NE = 16
DE = 2048
PAD = 256
GROUPS = ((1, 2048), (4, 512), (16, 128))
RECW = 1056
EPS = 1e-6


class Buf:
    def __init__(self, name):
        self.name = name
        self.w = {}
        self.r = {}
        self.dsem = None
        self.dcnt = 0


class StopBuild(Exception):
    pass


class Sched:
    nops = 0
    stop_at = None

    def __init__(self, nc, stack):
        self.nc = nc
        self.stack = stack
        self.eng = {"pe": nc.tensor, "act": nc.scalar, "dve": nc.vector,
                    "pool": nc.gpsimd, "sp": nc.sync}
        self.sem, self.cnt, self.known, self.allsems = {}, {}, {}, {}
        for k in self.eng:
            s = stack.enter_context(nc.semaphore("s_" + k))
            self.sem[k] = s
            self.cnt[k] = 0
            self.known[k] = {}
            self.allsems["s_" + k] = [s, 0]
        self.nbuf = 0
        self.bufs = []
        self.free_dsems = []

    def buf(self, name):
        self.nbuf += 1
        b = Buf("%s_%d" % (name, self.nbuf))
        self.bufs.append(b)
        return b

    def mark(self):
        return len(self.bufs)

    def release_since(self, mark):
        for b in self.bufs[mark:]:
            if b.dsem is not None:
                self.free_dsems.append((b.dsem, b.dcnt, b.dkey))
                b.dsem = None
        del self.bufs[mark:]

    @staticmethod
    def _need(needs, evs):
        for k, (s, v) in evs.items():
            if k not in needs or needs[k][1] < v:
                needs[k] = (s, v)

    def _waits(self, e, needs):
        eng = self.eng[e]
        kn = self.known[e]
        for k, (s, v) in needs.items():
            if e == "pe" and k == "s_pe":
                continue
            if kn.get(k, 0) < v:
                eng.wait_ge(s, v)
                kn[k] = v

    def op(self, e, fn, reads=(), writes=(), dma=None, inc=16):
        self.nops += 1
        if self.stop_at is not None and self.nops > self.stop_at:
            return {}
        needs = {}
        for b in reads:
            self._need(needs, b.w)
        for b in writes:
            self._need(needs, b.w)
            self._need(needs, b.r)
        self._waits(e, needs)
        ins = fn()
        if dma is not None:
            if dma.dsem is None:
                if self.free_dsems:
                    dma.dsem, dma.dcnt, dma.dkey = self.free_dsems.pop()
                else:
                    dma.dsem = self.stack.enter_context(self.nc.semaphore("d_" + dma.name))
                    dma.dkey = "d_" + dma.name
                    dma.dcnt = 0
                    self.allsems[dma.dkey] = [dma.dsem, 0]
            dma.dcnt += inc
            if inc == 1:
                ins.then_inc(dma.dsem)
            else:
                ins.then_inc(dma.dsem, inc)
            key = dma.dkey
            ev = (dma.dsem, dma.dcnt)
        else:
            self.cnt[e] += 1
            ins.then_inc(self.sem[e], 1)
            key = "s_" + e
            ev = (self.sem[e], self.cnt[e])
        self.allsems[key][1] = ev[1]
        for b in writes:
            b.w = {key: ev}
            b.r = {}
        for b in reads:
            if b in writes:
                continue
            b.r[key] = ev
        return {key: ev}

    def barrier(self):
        if self.stop_at is not None and self.nops > self.stop_at:
            if getattr(self, "_final", False):
                return
            self._final = True
        needs = {k: (s, v) for k, (s, v) in self.allsems.items() if v > 0}
        for e in self.eng:
            self._waits(e, needs)


def make_consts():
    p = np.arange(128)
    ident = np.eye(128, dtype=np.float32)
    gsel = (p[:, None] % 16 == p[None, :] % 16).astype(np.float32)
    iota = p.astype(np.float32)[:, None]
    caps = np.zeros((128, 2), np.float32)
    i16 = np.zeros((128, 16), np.float32)
    i16[:16] = np.eye(16)
    half = 8
    inv = np.power(np.float32(500000.0), -np.arange(half, dtype=np.float32) * np.float32(2.0 / 16))
    ang = np.arange(S_LEN, dtype=np.float32)[None, :] * inv[:, None]
    cosT = np.ones((128, S_LEN), np.float32)
    sinT = np.zeros((128, S_LEN), np.float32)
    pt = np.zeros((128, 128), np.float32)
    for h0 in (0, 64):
        for dd in range(16):
            cosT[h0 + dd] = np.cos(ang[dd % 8])
            sinT[h0 + dd] = np.sin(ang[dd % 8])
        for m in range(8):
            pt[h0 + m + 8, h0 + m] = -1.0
            pt[h0 + m, h0 + m + 8] = 1.0
    i = p[:, None]
    c = p[None, :]
    A = (c <= i).astype(np.float32)
    B = (c >= i).astype(np.float32)
    Af = A * (i >= 64)
    Bl = B * (i < 64)
    C = (np.abs(c - i) <= 64).astype(np.float32)
    masks = np.concatenate([Af, B, A, B, A, B, A, B, A, B, A, Bl, C, C, C, C], 1)
    U = (p[:, None] < p[None, :]).astype(np.float32)
    ones = np.ones((128, 128), np.float32)
    cstf = np.concatenate([ident, gsel, iota, i16], 1)
    cstb = np.concatenate([ident, pt, U, ones, masks, cosT, sinT], 1)
    return np.ascontiguousarray(cstf), np.ascontiguousarray(cstb)


def build(nP, nS, cap_slots, dbg=None, stop=None):
    NSEQ = nP + nS
    NT = NSEQ * S_LEN
    NTILE = NT // 128
    nPT = nP * 16
    CAPT = cap_slots // 128
    capP = 2 * (nP * NCORE * S_LEN) // NE
    capS = 2 * (nS * NCORE * S_LEN) // NE
    nc = bass.Bass("TRN2", target_bir_lowering=False)

    tiny = (dbg == "Bonly")

    def din(name, shape):
        if tiny and name.startswith("w_"):
            shape = [1] * len(shape)
        return nc.dram_tensor(name, shape, F32, kind="ExternalInput").ap()

    x_in = din("x_in", [NT, D])
    norm1_g = din("norm1_g", [DEPTH, D])
    w_in = din("w_in", [DEPTH, D, D_IN])
    conv_w = din("conv_w", [DEPTH, 3, 512])
    w_conv_out = din("w_conv_out", [DEPTH, 512, D])
    w_attn_out = din("w_attn_out", [DEPTH, 512, D])
    w_o = din("w_o", [DEPTH, D, D])
    norm2_g = din("norm2_g", [DEPTH, D])
    w_router = din("w_router", [DEPTH, D, NE])
    w_gate = din("w_gate", [DEPTH, NE, D, DE])
    w_up = din("w_up", [DEPTH, NE, D, DE])
    w_down = din("w_down", [DEPTH, NE, DE, D])
    final_g = din("final_g", [1, D])
    cstf_d = din("cstf", [128, 273])
    cstb_d = din("cstb", [128, 512 + 2048 + 4096])
    xs_init = din("xs_init", [cap_slots, RECW])
    capv_d = din("capv", [128, 2])
    y_out = nc.dram_tensor("y", [NT, D], F32, kind="ExternalOutput").ap()
    if dbg is not None:
        y1_out = nc.dram_tensor("y1", [NT, D], F32, kind="ExternalOutput").ap()
        dbg_out = nc.dram_tensor("dbgo", [128, 64], F32, kind="ExternalOutput").ap()

    xbuf = nc.dram_tensor("xbuf", [NT + 128, D], F32).ap()
    h2rec = nc.dram_tensor("h2rec", [NT, RECW], F32).ap()
    xs_d = [nc.dram_tensor("xs_d%d" % e, [cap_slots, RECW], F32).ap() for e in range(NE)]
    affT_d = nc.dram_tensor("affT_d", [NE, NT], F32).ap()
    gath_d = nc.dram_tensor("gath_d", [128, NT], F32).ap()

    with ExitStack() as st:
      S = Sched(nc, st)
      if isinstance(stop, int):
          S.stop_at = stop
      try:

            uid = [0]

            def sb(stack, name, shape, dt=F32):
                uid[0] += 1
                return stack.enter_context(nc.sbuf_tensor("sb_%s_%d" % (name, uid[0]), shape, dt))

            banks = [st.enter_context(nc.psum_tensor("bank%d" % i, [128, 512], F32)) for i in range(7)]
            BK = [S.buf("bank%d" % i) for i in range(7)]
            bankb = st.enter_context(nc.psum_tensor("bankb", [128, 1024], BF16))
            BKB = S.buf("bankb")
            rr = [0]

            def nextbank(lo=0, hi=3):
                i = lo + rr[0] % (hi - lo)
                rr[0] += 1
                return banks[i], BK[i]

            cstf = sb(st, "cstf", [128, 273]); CSTF = S.buf("cstf")
            cb = sb(st, "cb", [128, 512 + 2048 + 4096], BF16); CB = S.buf("cb")
            S.op("sp", lambda: nc.sync.dma_start(out=cstf[:], in_=cstf_d[:, :]), writes=[CSTF], dma=CSTF)
            for q in range(0, 6656, 1664):
                S.op("pool", lambda q=q: nc.gpsimd.dma_start(out=cb[:, q:q + 1664], in_=cstb_d[:, q:q + 1664]),
                     writes=[CB], dma=CB)
            identf = cstf[:, 0:128]
            gsel = cstf[:, 128:256]
            iota = cstf[:, 256:257]
            i16 = cstf[0:16, 257:273]
            identb = cb[:, 0:128]
            ptb = cb[:, 128:256]
            Ub = cb[:, 256:384]
            onesb = cb[:, 384:512]
            maskb = [cb[:, 512 + 512 * k: 1024 + 512 * k] for k in range(4)]
            cosb = cb[:, 2560:2560 + 2048]
            sinb = cb[:, 4608:4608 + 2048]
            capv = sb(st, "capv", [128, 2]); CAPV = S.buf("capv")
            S.op("sp", lambda: nc.sync.dma_start(out=capv[:], in_=capv_d[:, :]), writes=[CAPV], dma=CAPV)

            aff_all = sb(st, "aff_all", [128, NTILE, NE]); AFF = S.buf("aff")
            small = sb(st, "small", [128, 64]); SM = [S.buf("sm%d" % i) for i in range(64)]
            wrr = [0]

            bc_slots = nc.gpsimd.to_reg(cap_slots - 1)
            bc_rows = nc.gpsimd.to_reg(NT + 127)
            XB = [S.buf("xbuf%d" % s) for s in range(NSEQ)]
            H2R = S.buf("h2rec")
            XIN = S.buf("xin")

            def wv(l):
                return w_in[l].rearrange("(kc k) c -> k kc c", k=128)

            def rstd_ops(ss, rs, SSB, RSB):
                S.op("dve", lambda: nc.vector.tensor_scalar(out=rs, in0=ss, scalar1=1.0 / D, scalar2=EPS,
                                                             op0=ALU.mult, op1=ALU.add), reads=[SSB], writes=[RSB])
                S.op("act", lambda: nc.scalar.activation(out=rs, in_=rs, func=AF.Sqrt), reads=[RSB], writes=[RSB])
                S.op("dve", lambda: nc.vector.reciprocal(out=rs, in_=rs), reads=[RSB], writes=[RSB])

            for l in range(DEPTH):
                if tiny:
                    with ExitStack() as la:
                        xt0 = sb(la, "xt0", [128, NE]); XT0 = S.buf("xt0")
                        for gt in range(NTILE):
                            S.op("sp", lambda gt=gt: nc.sync.dma_start(out=xt0[:], in_=x_in[gt * 128:(gt + 1) * 128, 0:NE]), writes=[XT0], dma=XT0)
                            S.op("act", lambda gt=gt: nc.scalar.activation(out=aff_all[:, gt, :], in_=xt0[:], func=AF.Sigmoid), reads=[XT0], writes=[AFF])
                        S.barrier()
                mk_layer = S.mark()
                with ExitStack() as la:
                  if not tiny:
                    gbc = sb(la, "gbc", [128, D]); GBC = S.buf("gbc")
                    g2bc = sb(la, "g2bc", [128, D]); G2BC = S.buf("g2bc")
                    wbufs = [sb(la, "wb%d" % i, [128, 8, 3, 128], BF16) for i in range(2)]
                    WB = [S.buf("wb%d" % i) for i in range(2)]

                    def nextw():
                        i = wrr[0] % 2
                        wrr[0] += 1
                        return wbufs[i], WB[i]

                    hT = sb(la, "hT", [128, 8, S_LEN + 2 * PAD], BF16)
                    HT = [S.buf("hT%d" % i) for i in range(16)]
                    HTP = S.buf("hTpad")
                    ucT = sb(la, "ucT", [128, 4, S_LEN], BF16); UCT = [S.buf("ucT%d" % j) for j in range(4)]
                    oT = sb(la, "oT", [128, 4, S_LEN], BF16); OT = [S.buf("oT%d" % j) for j in range(4)]
                    wo = sb(la, "wo", [128, 8, D], BF16); WO = S.buf("wo")
                    wrg = sb(la, "wrg", [128, 8, NE]); WRG = S.buf("wrg")
                    cw = sb(la, "cw", [128, 3, 4]); CW = S.buf("cw")
                    xts = [sb(la, "xt%d" % i, [128, D]) for i in range(2)]
                    XT = [S.buf("xt%d" % i) for i in range(2)]
                    junk = sb(la, "junk", [128, D], BF16); JUNK = S.buf("junk")

                    S.op("pool", lambda: nc.gpsimd.memset(hT[:, :, 0:PAD], 0.0), writes=[HTP])
                    S.op("pool", lambda: nc.gpsimd.memset(hT[:, :, PAD + S_LEN:], 0.0), writes=[HTP])
                    for kc in range(8):
                        S.op("pool", lambda kc=kc: nc.gpsimd.dma_start(out=wo[:, kc, :], in_=w_o[l, kc * 128:(kc + 1) * 128, :]),
                             writes=[WO], dma=WO)
                    S.op("sp", lambda: nc.sync.dma_start(out=gbc[:], in_=norm1_g[l:l + 1, :].to_broadcast([128, D])),
                         writes=[GBC], dma=GBC)
                    S.op("sp", lambda: nc.sync.dma_start(out=g2bc[:], in_=norm2_g[l:l + 1, :].to_broadcast([128, D])),
                         writes=[G2BC], dma=G2BC)
                    with nc.allow_non_contiguous_dma(reason="tiny per-layer vectors"):
                        for w3 in range(3):
                            S.op("sp", lambda w3=w3: nc.sync.dma_start(out=cw[:, w3, :], in_=conv_w[l, w3].rearrange("(j c) -> c j", c=128)),
                                 writes=[CW], dma=CW)
                        S.op("sp", lambda: nc.sync.dma_start(out=wrg[:], in_=w_router[l].rearrange("(kc k) e -> k kc e", k=128)),
                             writes=[WRG], dma=WRG)
                        g2col = sb(la, "g2col", [128, 8]); G2C = S.buf("g2col")
                        S.op("sp", lambda: nc.sync.dma_start(out=g2col[:], in_=norm2_g[l].rearrange("(kc k) -> k kc", k=128)),
                             writes=[G2C], dma=G2C)
                    for kc in range(8):
                        S.op("dve", lambda kc=kc: nc.vector.tensor_scalar(out=wrg[:, kc, :], in0=wrg[:, kc, :],
                                                                           scalar1=g2col[:, kc:kc + 1], scalar2=None, op0=ALU.mult),
                             reads=[G2C], writes=[WRG])

                    for s in range(NSEQ):
                        mk_seq = S.mark()
                        r0 = s * S_LEN
                        xsrc = x_in if l == 0 else xbuf
                        XSRC = XIN if l == 0 else XB[s]
                        with ExitStack() as ph:
                            hbs = [sb(ph, "hb%d" % i, [128, D], BF16) for i in range(2)]
                            HB = [S.buf("hb%d" % i) for i in range(2)]
                            for tt in range(16):
                                xt, XTb = xts[tt % 2], XT[tt % 2]
                                hb, HBb = hbs[tt % 2], HB[tt % 2]
                                ss, rs = small[:, 0:1], small[:, 1:2]
                                S.op("sp", lambda: nc.sync.dma_start(out=xt[:], in_=xsrc[r0 + tt * 128: r0 + (tt + 1) * 128, :]),
                                     reads=[XSRC], writes=[XTb], dma=XTb)
                                S.op("act", lambda: nc.scalar.activation(out=junk[:], in_=xt[:], func=AF.Square, accum_out=ss),
                                     reads=[XTb], writes=[JUNK, SM[0]])
                                rstd_ops(ss, rs, SM[0], SM[1])
                                S.op("dve", lambda: nc.vector.scalar_tensor_tensor(out=hb[:], in0=xt[:], scalar=rs, in1=gbc[:],
                                                                                    op0=ALU.mult, op1=ALU.mult),
                                     reads=[XTb, SM[1], GBC], writes=[HBb])
                                for kc in range(8):
                                    S.op("pe", lambda kc=kc: nc.tensor.transpose(out=bankb[:, kc * 128:(kc + 1) * 128],
                                                                                  in_=hb[:, kc * 128:(kc + 1) * 128], identity=identb),
                                         reads=[HBb, CB], writes=[BKB])
                                S.op("act", lambda: nc.scalar.copy(out=hT[:, :, PAD + tt * 128: PAD + (tt + 1) * 128],
                                                                   in_=bankb[:, :].rearrange("p (k t) -> p k t", k=8)),
                                     reads=[BKB], writes=[HT[tt]])
                        S.barrier()
                        HTall = HT + [HTP]
                        with ExitStack() as ph:
                            zc = sb(ph, "zc", [128, 3, S_LEN], BF16); ZC = [S.buf("zc%d" % i) for i in range(3)]
                            u = sb(ph, "u", [128, S_LEN + 2]); UU = S.buf("u")
                            yv = sb(ph, "yv", [128, S_LEN]); YV = S.buf("yv")
                            S.op("pool", lambda: nc.gpsimd.memset(u[:, 0:1], 0.0), writes=[UU])
                            S.op("pool", lambda: nc.gpsimd.memset(u[:, S_LEN + 1:S_LEN + 2], 0.0), writes=[UU])
                            for j in range(4):
                                wt, WTb = nextw()
                                for t3 in range(3):
                                    c0 = t3 * 512 + j * 128
                                    S.op("pool", lambda t3=t3, c0=c0: nc.gpsimd.dma_start(out=wt[:, :, t3, :], in_=wv(l)[:, :, c0:c0 + 128]),
                                         writes=[WTb], dma=WTb)
                                for t3 in range(3):
                                    for tc in range(4):
                                        bk, BKb = nextbank()
                                        for kc in range(8):
                                            S.op("pe", lambda kc=kc, t3=t3, tc=tc, bk=bk: nc.tensor.matmul(
                                                bk[:, :], lhsT=wt[:, kc, t3, :], rhs=hT[:, kc, PAD + tc * 512: PAD + (tc + 1) * 512],
                                                start=(kc == 0), stop=(kc == 7)), reads=[WTb] + HTall, writes=[BKb])
                                        S.op("act", lambda t3=t3, tc=tc, bk=bk: nc.scalar.copy(out=zc[:, t3, tc * 512:(tc + 1) * 512], in_=bk[:, :]),
                                             reads=[BKb], writes=[ZC[t3]])
                                S.op("dve", lambda: nc.vector.tensor_tensor(out=u[:, 1:S_LEN + 1], in0=zc[:, 1, :], in1=zc[:, 2, :], op=ALU.mult),
                                     reads=[ZC[1], ZC[2]], writes=[UU])
                                S.op("dve", lambda j=j: nc.vector.tensor_scalar(out=yv[:], in0=u[:, 1:S_LEN + 1], scalar1=cw[:, 1, j:j + 1],
                                                                                 scalar2=None, op0=ALU.mult), reads=[UU, CW], writes=[YV])
                                S.op("dve", lambda j=j: nc.vector.scalar_tensor_tensor(out=yv[:], in0=u[:, 0:S_LEN], scalar=cw[:, 0, j:j + 1],
                                                                                        in1=yv[:], op0=ALU.mult, op1=ALU.add),
                                     reads=[UU, CW], writes=[YV])
                                S.op("dve", lambda j=j: nc.vector.scalar_tensor_tensor(out=yv[:], in0=u[:, 2:S_LEN + 2], scalar=cw[:, 2, j:j + 1],
                                                                                        in1=yv[:], op0=ALU.mult, op1=ALU.add),
                                     reads=[UU, CW], writes=[YV])
                                S.op("dve", lambda j=j: nc.vector.tensor_tensor(out=ucT[:, j, :], in0=zc[:, 0, :], in1=yv[:], op=ALU.mult),
                                     reads=[ZC[0], YV], writes=[UCT[j]])
                        S.barrier()
                        with ExitStack() as ph:
                            accn = sb(ph, "accn", [64, 2, S_LEN]); ACCN = [S.buf("accn%d" % i) for i in range(2)]
                            accd = sb(ph, "accd", [64, 2, S_LEN]); ACCD = [S.buf("accd%d" % i) for i in range(2)]
                            qk = sb(ph, "qk", [128, 2, S_LEN + 2 * PAD], BF16); QK = [S.buf("qk%d" % i) for i in range(2)]
                            qraw = [sb(ph, "qraw%d" % i, [128, 512], BF16) for i in range(2)]
                            QR = [S.buf("qraw%d" % i) for i in range(2)]
                            rt1s = [sb(ph, "rt1_%d" % i, [128, 512]) for i in range(2)]; RT1S = [S.buf("rt1_%d" % i) for i in range(2)]
                            rt2s = [sb(ph, "rt2_%d" % i, [128, 512]) for i in range(2)]; RT2S = [S.buf("rt2_%d" % i) for i in range(2)]
                            v_sb = sb(ph, "v_sb", [128, 20, 128], BF16); VS = S.buf("v_sb")
                            p_sb = [sb(ph, "p_sb%d" % i, [128, 512], BF16) for i in range(2)]
                            PS = [S.buf("p_sb%d" % i) for i in range(2)]
                            pm = [sb(ph, "pm%d" % i, [128, 512], BF16) for i in range(2)]
                            PM = [S.buf("pm%d" % i) for i in range(2)]
                            S.op("pool", lambda: nc.gpsimd.memset(qk[:, :, 0:PAD], 0.0), writes=QK)
                            S.op("pool", lambda: nc.gpsimd.memset(qk[:, :, PAD + S_LEN:], 0.0), writes=QK)
                            NUMB, NUMBb = banks[4], BK[4]
                            DENB, DENBb = banks[5], BK[5]
                            cnt = [0]
                            for hp in range(4):
                                for g, (dil, L) in enumerate(GROUPS):
                                    wt, WTb = nextw()
                                    for t3, off in enumerate((OFF_Q, OFF_K, OFF_V)):
                                        c0 = off + g * 512 + hp * 128
                                        S.op("pool", lambda t3=t3, c0=c0: nc.gpsimd.dma_start(out=wt[:, :, t3, :], in_=wv(l)[:, :, c0:c0 + 128]),
                                             writes=[WTb], dma=WTb)
                                    for t3 in range(2):
                                        for tc in range(4):
                                            bk, BKb = nextbank()
                                            qr, QRb = qraw[tc % 2], QR[tc % 2]
                                            rt1, RT1 = rt1s[tc % 2], RT1S[tc % 2]
                                            rt2, RT2 = rt2s[tc % 2], RT2S[tc % 2]
                                            for kc in range(8):
                                                S.op("pe", lambda kc=kc, t3=t3, tc=tc, bk=bk: nc.tensor.matmul(
                                                    bk[:, :], lhsT=wt[:, kc, t3, :], rhs=hT[:, kc, PAD + tc * 512: PAD + (tc + 1) * 512],
                                                    start=(kc == 0), stop=(kc == 7)), reads=[WTb] + HTall, writes=[BKb])
                                            S.op("act", lambda bk=bk, qr=qr: nc.scalar.copy(out=qr[:], in_=bk[:, :]), reads=[BKb], writes=[QRb])
                                            bk2, BK2b = nextbank()
                                            S.op("pe", lambda bk2=bk2, qr=qr: nc.tensor.matmul(bk2[:, :], lhsT=ptb, rhs=qr[:], start=True, stop=True),
                                                 reads=[QRb, CB], writes=[BK2b])
                                            S.op("dve", lambda qr=qr, tc=tc, rt1=rt1: nc.vector.tensor_tensor(out=rt1[:], in0=qr[:], in1=cosb[:, tc * 512:(tc + 1) * 512],
                                                                                                      op=ALU.mult), reads=[QRb, CB], writes=[RT1])
                                            S.op("dve", lambda bk2=bk2, tc=tc, rt2=rt2: nc.vector.tensor_tensor(out=rt2[:], in0=bk2[:, :], in1=sinb[:, tc * 512:(tc + 1) * 512],
                                                                                                        op=ALU.mult), reads=[BK2b, CB], writes=[RT2])
                                            S.op("dve", lambda t3=t3, tc=tc, rt1=rt1, rt2=rt2: nc.vector.tensor_tensor(out=qk[:, t3, PAD + tc * 512: PAD + (tc + 1) * 512],
                                                                                                      in0=rt1[:], in1=rt2[:], op=ALU.add),
                                                 reads=[RT1, RT2], writes=[QK[t3]])
                                    if g < 2:
                                        ktiles = [(r, m, PAD + r + dil * (128 * m - 64)) for r in range(dil) for m in range(L // 128 + 1)]
                                    else:
                                        ktiles = [(r, 0, PAD + r) for r in range(16)]
                                    for t0 in range(0, len(ktiles), 4):
                                        bk, BKb = nextbank()
                                        grp = ktiles[t0:t0 + 4]
                                        for si, (r, m, st0) in enumerate(grp):
                                            for kc in range(8):
                                                S.op("pe", lambda kc=kc, si=si, st0=st0, bk=bk: nc.tensor.matmul(
                                                    bk[:, si * 128:(si + 1) * 128], lhsT=hT[:, kc, st0: st0 + 127 * dil + 1: dil], rhs=wt[:, kc, 2, :],
                                                    start=(kc == 0), stop=(kc == 7)), reads=[WTb] + HTall, writes=[BKb])
                                        n = len(grp)
                                        S.op("act", lambda bk=bk, t0=t0, n=n: nc.scalar.copy(
                                            out=v_sb[:, t0:t0 + n, :], in_=bk[:, 0:n * 128].rearrange("p (s c) -> p s c", c=128)),
                                            reads=[BKb], writes=[VS])
                                    jobs = []
                                    for hh in range(2):
                                        h0 = hh * 64
                                        for quad in range(4):
                                            if g < 2:
                                                r = 0 if g == 0 else quad
                                                jq = [4 * quad + i for i in range(4)] if g == 0 else [0, 1, 2, 3]
                                                nm = L // 128 + 1
                                                if g == 0:
                                                    nview = accn[:, hh, quad * 512:(quad + 1) * 512]
                                                    dview = accd[:, hh, quad * 512:(quad + 1) * 512]
                                                else:
                                                    nview = accn[:, hh, quad: S_LEN: 4]
                                                    dview = accd[:, hh, quad: S_LEN: 4]
                                                nsrc, dsrc = NUMB[0:64, :], DENB[0:64, :]
                                                for half in range(2):
                                                    sc, pv = [], []
                                                    for qq in range(2):
                                                        j = jq[2 * half + qq]
                                                        qs = PAD + r + dil * 128 * j
                                                        osl = (2 * half + qq) * 128
                                                        for ab in range(2):
                                                            ks = PAD + r + dil * (128 * (j + ab) - 64)
                                                            sl = (2 * qq + ab) * 128
                                                            sc.append((sl, ks, qs, dil))
                                                            pv.append((osl, sl, r * nm + j + ab, ab == 0, ab == 1))
                                                    first = (jq[2 * half] == 0)
                                                    last = (jq[2 * half + 1] == L // 128 - 1)
                                                    mk = maskb[0] if first else (maskb[2] if last else maskb[1])
                                                    jobs.append(dict(h0=h0, hh=hh, sc=sc, pv=pv, mk=mk,
                                                                     post=(nview, dview, nsrc, dsrc) if half == 1 else None))
                                            else:
                                                sc, pv = [], []
                                                for k4 in range(4):
                                                    r = 4 * quad + k4
                                                    sc.append((k4 * 128, PAD + r, PAD + r, 16))
                                                    pv.append((k4 * 128, k4 * 128, r, True, True))
                                                nview = accn[:, hh, :].rearrange("p (c s) -> p s c", s=16)[:, 4 * quad:4 * quad + 4, :]
                                                dview = accd[:, hh, :].rearrange("p (c s) -> p s c", s=16)[:, 4 * quad:4 * quad + 4, :]
                                                nsrc = NUMB[0:64, :].rearrange("p (s c) -> p s c", c=128)
                                                dsrc = DENB[0:64, :].rearrange("p (s c) -> p s c", c=128)
                                                jobs.append(dict(h0=h0, hh=hh, sc=sc, pv=pv, mk=maskb[3], post=(nview, dview, nsrc, dsrc)))

                                    def emit_scores(k):
                                        jb = jobs[k]
                                        h0 = jb["h0"]
                                        sbk, SBKb = (banks[6], BK[6]) if k % 2 == 0 else (banks[3], BK[3])
                                        pp, PPb = p_sb[k % 2], PS[k % 2]
                                        pmm, PMb = pm[k % 2], PM[k % 2]
                                        for (sl, ks, qs, dd) in jb["sc"]:
                                            S.op("pe", lambda sl=sl, ks=ks, qs=qs, dd=dd, sbk=sbk, h0=h0: nc.tensor.matmul(
                                                sbk[:, sl:sl + 128], lhsT=qk[h0:h0 + 64, 1, ks: ks + 127 * dd + 1: dd],
                                                rhs=qk[h0:h0 + 64, 0, qs: qs + 127 * dd + 1: dd], start=True, stop=True), reads=QK, writes=[SBKb])
                                        S.op("act", lambda sbk=sbk, pp=pp: nc.scalar.activation(out=pp[:], in_=sbk[:, :], func=AF.Exp, scale=0.125),
                                             reads=[SBKb], writes=[PPb])
                                        S.op("dve", lambda pp=pp, pmm=pmm, mk=jb["mk"]: nc.vector.tensor_tensor(out=pmm[:], in0=pp[:], in1=mk, op=ALU.mult),
                                             reads=[PPb, CB], writes=[PMb])

                                    def emit_pv(k):
                                        jb = jobs[k]
                                        h0, hh = jb["h0"], jb["hh"]
                                        pmm, PMb = pm[k % 2], PM[k % 2]
                                        for (osl, sl, vt, st_, sp_) in jb["pv"]:
                                            S.op("pe", lambda osl=osl, sl=sl, vt=vt, st_=st_, sp_=sp_, pmm=pmm, h0=h0: nc.tensor.matmul(
                                                NUMB[0:64, osl:osl + 128], lhsT=v_sb[:, vt, h0:h0 + 64], rhs=pmm[:, sl:sl + 128],
                                                start=st_, stop=sp_, skip_group_check=True), reads=[VS, PMb], writes=[NUMBb])
                                            S.op("pe", lambda osl=osl, sl=sl, st_=st_, sp_=sp_, pmm=pmm: nc.tensor.matmul(
                                                DENB[0:64, osl:osl + 128], lhsT=onesb[:, 0:64], rhs=pmm[:, sl:sl + 128],
                                                start=st_, stop=sp_, skip_group_check=True), reads=[CB, PMb], writes=[DENBb])
                                        if jb["post"] is not None:
                                            nview, dview, nsrc, dsrc = jb["post"]
                                            if g == 0:
                                                S.op("act", lambda nview=nview, nsrc=nsrc: nc.scalar.copy(out=nview, in_=nsrc), reads=[NUMBb], writes=[ACCN[hh]])
                                                S.op("dve", lambda dview=dview, dsrc=dsrc: nc.vector.tensor_copy(out=dview, in_=dsrc), reads=[DENBb], writes=[ACCD[hh]])
                                            else:
                                                S.op("dve", lambda nview=nview, nsrc=nsrc: nc.vector.tensor_tensor(out=nview, in0=nview, in1=nsrc, op=ALU.add),
                                                     reads=[NUMBb], writes=[ACCN[hh]])
                                                S.op("dve", lambda dview=dview, dsrc=dsrc: nc.vector.tensor_tensor(out=dview, in0=dview, in1=dsrc, op=ALU.add),
                                                     reads=[DENBb], writes=[ACCD[hh]])

                                    emit_scores(0)
                                    for k in range(len(jobs)):
                                        if k + 1 < len(jobs):
                                            emit_scores(k + 1)
                                        emit_pv(k)
                                for hh in range(2):
                                    S.op("dve", lambda hh=hh: nc.vector.reciprocal(out=accd[:, hh, :], in_=accd[:, hh, :]), writes=[ACCD[hh]])
                                    S.op("dve", lambda hh=hh, hp=hp: nc.vector.tensor_tensor(out=oT[hh * 64:(hh + 1) * 64, hp, :], in0=accn[:, hh, :],
                                                                                             in1=accd[:, hh, :], op=ALU.mult),
                                         reads=[ACCN[hh], ACCD[hh]], writes=[OT[hp]])
                        S.barrier()
                        with ExitStack() as ph:
                            mT = sb(ph, "mT", [128, 8, 1024], BF16); MT = S.buf("mT")
                            sg = [sb(ph, "sg%d" % i, [128, 512]) for i in range(2)]; SG = [S.buf("sg%d" % i) for i in range(2)]
                            m1 = sb(ph, "m1", [128, 512]); M1 = S.buf("m1")
                            m2 = sb(ph, "m2", [128, 512]); M2 = S.buf("m2")
                            x1 = [sb(ph, "x1_%d" % i, [128, D]) for i in range(2)]; X1 = [S.buf("x1_%d" % i) for i in range(2)]
                            rec = [sb(ph, "rec%d" % i, [128, RECW]) for i in range(2)]; REC = [S.buf("rec%d" % i) for i in range(2)]
                            x1T = sb(ph, "x1T", [128, 8, 128]); X1T = S.buf("x1T")
                            lg = sb(ph, "lg", [128, NE]); LG = S.buf("lg")
                            for hs in range(2):
                                for dc in range(8):
                                    wt, WTb = nextw()
                                    for t3 in range(2):
                                        c0 = OFF_G + t3 * 1024 + dc * 128
                                        S.op("pool", lambda t3=t3, c0=c0: nc.gpsimd.dma_start(out=wt[:, :, t3, :], in_=wv(l)[:, :, c0:c0 + 128]),
                                             writes=[WTb], dma=WTb)
                                    S.op("pool", lambda dc=dc: nc.gpsimd.dma_start(
                                        out=wt[:, 0:4, 2, :], in_=w_conv_out[l].rearrange("(kc k) c -> k kc c", k=128)[:, :, dc * 128:(dc + 1) * 128]),
                                        writes=[WTb], dma=WTb)
                                    S.op("pool", lambda dc=dc: nc.gpsimd.dma_start(
                                        out=wt[:, 4:8, 2, :], in_=w_attn_out[l].rearrange("(kc k) c -> k kc c", k=128)[:, :, dc * 128:(dc + 1) * 128]),
                                        writes=[WTb], dma=WTb)
                                    for tcl in range(2):
                                        tok0 = hs * 1024 + tcl * 512
                                        gb = []
                                        for t3 in range(2):
                                            bk, BKb = nextbank()
                                            gb.append((bk, BKb))
                                            for kc in range(8):
                                                S.op("pe", lambda kc=kc, t3=t3, bk=bk: nc.tensor.matmul(
                                                    bk[:, :], lhsT=wt[:, kc, t3, :], rhs=hT[:, kc, PAD + tok0: PAD + tok0 + 512],
                                                    start=(kc == 0), stop=(kc == 7)), reads=[WTb] + HTall, writes=[BKb])
                                            S.op("act", lambda t3=t3, bk=bk: nc.scalar.activation(out=sg[t3][:], in_=bk[:, :], func=AF.Sigmoid),
                                                 reads=[BKb], writes=[SG[t3]])
                                        bkc, BKc = nextbank()
                                        for kc in range(4):
                                            S.op("pe", lambda kc=kc, bkc=bkc: nc.tensor.matmul(
                                                bkc[:, :], lhsT=wt[:, kc, 2, :], rhs=ucT[:, kc, tok0:tok0 + 512], start=(kc == 0), stop=(kc == 3)),
                                                reads=[WTb] + UCT, writes=[BKc])
                                        bka, BKa = nextbank()
                                        for kc in range(4):
                                            S.op("pe", lambda kc=kc, bka=bka: nc.tensor.matmul(
                                                bka[:, :], lhsT=wt[:, 4 + kc, 2, :], rhs=oT[:, kc, tok0:tok0 + 512], start=(kc == 0), stop=(kc == 3)),
                                                reads=[WTb] + OT, writes=[BKa])
                                        S.op("dve", lambda bkc=bkc: nc.vector.tensor_tensor(out=m1[:], in0=sg[0][:], in1=bkc[:, :], op=ALU.mult),
                                             reads=[SG[0], BKc], writes=[M1])
                                        S.op("dve", lambda bka=bka: nc.vector.tensor_tensor(out=m2[:], in0=sg[1][:], in1=bka[:, :], op=ALU.mult),
                                             reads=[SG[1], BKa], writes=[M2])
                                        S.op("dve", lambda dc=dc, tcl=tcl: nc.vector.tensor_tensor(out=mT[:, dc, tcl * 512:(tcl + 1) * 512], in0=m1[:], in1=m2[:],
                                                                                                  op=ALU.add), reads=[M1, M2], writes=[MT])
                                for tt in range(8):
                                    tok = hs * 1024 + tt * 128
                                    gt = s * 16 + hs * 8 + tt
                                    xt, XTb = xts[tt % 2], XT[tt % 2]
                                    xx, XXb = x1[tt % 2], X1[tt % 2]
                                    rc, RCb = rec[tt % 2], REC[tt % 2]
                                    ss, rs, mx, se = small[:, 2:3], small[:, 3:4], small[:, 4:5], small[:, 5:6]
                                    S.op("sp", lambda: nc.sync.dma_start(out=xt[:], in_=xsrc[r0 + tok: r0 + tok + 128, :]),
                                         reads=[XSRC], writes=[XTb], dma=XTb)
                                    for half in range(2):
                                        bk, BKb = nextbank()
                                        for kc in range(8):
                                            S.op("pe", lambda kc=kc, half=half, bk=bk: nc.tensor.matmul(
                                                bk[:, :], lhsT=mT[:, kc, tt * 128:(tt + 1) * 128], rhs=wo[:, kc, half * 512:(half + 1) * 512],
                                                start=(kc == 0), stop=(kc == 7)), reads=[MT, WO], writes=[BKb])
                                        S.op("dve", lambda half=half, bk=bk: nc.vector.tensor_tensor(
                                            out=xx[:, half * 512:(half + 1) * 512], in0=xt[:, half * 512:(half + 1) * 512], in1=bk[:, :], op=ALU.add),
                                            reads=[XTb, BKb], writes=[XXb])
                                    S.op("sp", lambda: nc.sync.dma_start(out=xbuf[r0 + tok: r0 + tok + 128, :], in_=xx[:]),
                                         reads=[XXb], writes=[XB[s]], dma=XXb)
                                    S.op("act", lambda: nc.scalar.activation(out=junk[:], in_=xx[:], func=AF.Square, accum_out=ss),
                                         reads=[XXb], writes=[JUNK, SM[2]])
                                    rstd_ops(ss, rs, SM[2], SM[3])
                                    for q4 in range(2):
                                        bk, BKb = nextbank()
                                        for k4 in range(4):
                                            kc = q4 * 4 + k4
                                            S.op("pe", lambda kc=kc, k4=k4, bk=bk: nc.tensor.transpose(
                                                out=bk[:, k4 * 128:(k4 + 1) * 128], in_=xx[:, kc * 128:(kc + 1) * 128], identity=identf),
                                                reads=[XXb, CSTF], writes=[BKb])
                                        S.op("act", lambda q4=q4, bk=bk: nc.scalar.copy(out=x1T[:, q4 * 4:(q4 + 1) * 4, :],
                                                                                        in_=bk[:, :].rearrange("p (k t) -> p k t", k=4)),
                                             reads=[BKb], writes=[X1T])
                                    bk, BKb = nextbank()
                                    for kc in range(8):
                                        S.op("pe", lambda kc=kc, bk=bk: nc.tensor.matmul(bk[:, 0:NE], lhsT=x1T[:, kc, :], rhs=wrg[:, kc, :],
                                                                                         start=(kc == 0), stop=(kc == 7)), reads=[X1T, WRG], writes=[BKb])
                                    S.op("dve", lambda bk=bk: nc.vector.tensor_scalar(out=lg[:], in0=bk[:, 0:NE], scalar1=rs, scalar2=None, op0=ALU.mult),
                                         reads=[BKb, SM[3]], writes=[LG])
                                    S.op("dve", lambda: nc.vector.reduce_max(out=mx, in_=lg[:], axis=AX.X), reads=[LG], writes=[SM[4]])
                                    S.op("dve", lambda: nc.vector.tensor_scalar(out=mx, in0=mx, scalar1=-1.0, scalar2=None, op0=ALU.mult),
                                         reads=[SM[4]], writes=[SM[4]])
                                    S.op("act", lambda: nc.scalar.activation(out=lg[:], in_=lg[:], func=AF.Exp, bias=mx, accum_out=se),
                                         reads=[SM[4]], writes=[LG, SM[5]])
                                    S.op("dve", lambda: nc.vector.reciprocal(out=se, in_=se), writes=[SM[5]])
                                    S.op("dve", lambda gt=gt: nc.vector.tensor_scalar(out=aff_all[:, gt, :], in0=lg[:], scalar1=se, scalar2=None, op0=ALU.mult),
                                         reads=[LG, SM[5]], writes=[AFF])
                                    S.op("dve", lambda: nc.vector.scalar_tensor_tensor(out=rc[:, 0:D], in0=xx[:], scalar=rs, in1=g2bc[:],
                                                                                        op0=ALU.mult, op1=ALU.mult),
                                         reads=[XXb, SM[3], G2BC], writes=[RCb])
                                    S.op("act", lambda gt=gt: nc.scalar.copy(out=rc[:, D:D + NE], in_=aff_all[:, gt, :]), reads=[AFF], writes=[RCb])
                                    S.op("dve", lambda gt=gt: nc.vector.tensor_scalar(out=rc[:, D + NE:D + NE + 1], in0=iota, scalar1=float(gt * 128),
                                                                                       scalar2=None, op0=ALU.add), reads=[CSTF], writes=[RCb])
                                    S.op("sp", lambda gt=gt: nc.sync.dma_start(out=h2rec[gt * 128:(gt + 1) * 128, :], in_=rc[:]),
                                         reads=[RCb], writes=[H2R], dma=RCb)
                        S.barrier()
                        S.release_since(mk_seq)
                S.barrier()
                S.release_since(mk_layer)
                if dbg == "A":
                    DBG = S.buf("dbg")
                    for q0 in range(0, NT, 2048):
                        S.op("sp", lambda q0=q0: nc.sync.dma_start(out=y_out[q0:q0 + 2048, :], in_=xbuf[q0:q0 + 2048, :]), writes=[DBG], dma=DBG)
                    S.barrier()
                    return nc
                if dbg is not None and l == 0:
                    DBG = S.buf("dbg")
                    for q0 in range(0, NT, 2048):
                        S.op("sp", lambda q0=q0: nc.sync.dma_start(out=y1_out[q0:q0 + 2048, :], in_=xbuf[q0:q0 + 2048, :]), writes=[DBG], dma=DBG)
                    S.barrier()
                XSD = S.buf("xs_d")
                XBALL = S.buf("xball")
                mk_b = S.mark()
                with ExitStack() as lb:
                    G = sb(lb, "G", [128, NT]); GB = S.buf("G")
                    stg = sb(lb, "stg", [NE, 512]); STG = S.buf("stg")
                    cmpj = sb(lb, "cmpj", [128, max(nP, nS) * S_LEN], BF16); CMPJ = S.buf("cmpj")
                    bis = sb(lb, "bis", [128, 16]); BIS = S.buf("bis")
                    thb = sb(lb, "thb", [NE, 2, 128]); THB = S.buf("thb")
                    thr = sb(lb, "thr", [128, 2, NE]); THR = S.buf("thr")
                    Mk = sb(lb, "Mk", [128, NTILE, NE]); MK = S.buf("Mk")
                    Mkb = sb(lb, "Mkb", [128, NTILE * NE], BF16); MKB = S.buf("Mkb")
                    pre = sb(lb, "pre", [128, NTILE, NE]); PRE = S.buf("pre")
                    tot = sb(lb, "tot", [128, NTILE, NE]); TOT = S.buf("tot")
                    cum = sb(lb, "cum", [128, NTILE, NE]); CUM = S.buf("cum")
                    onesf = sb(lb, "onesf", [128, 128]); ONF = S.buf("onesf")
                    hs2 = sb(lb, "hs2", [128, NTILE, NE]); HS2 = S.buf("hs2")
                    cum2 = sb(lb, "cum2", [128, NTILE, NE]); CUM2 = S.buf("cum2")
                    idxi = sb(lb, "idxi", [128, NTILE, NE], I32); IDX = S.buf("idxi")
                    recl = [sb(lb, "recl%d" % i, [128, RECW]) for i in range(3)]; RECL = [S.buf("recl%d" % i) for i in range(3)]
                    AFT = S.buf("affT_d"); GTH = S.buf("gath_d")
                    for e in range(NE):
                        S.op("sp", lambda e=e: nc.sync.dma_start(out=xs_d[e][:, :], in_=xs_init[:, :]), writes=[XSD], dma=XSD)
                    for t0 in range(0, NTILE, 4):
                        bk, BKb = nextbank()
                        for k4 in range(4):
                            S.op("pe", lambda k4=k4, t0=t0, bk=bk: nc.tensor.transpose(out=bk[0:NE, k4 * 128:(k4 + 1) * 128],
                                                                                       in_=aff_all[:, t0 + k4, :], identity=identf),
                                 reads=[AFF, CSTF], writes=[BKb])
                        S.op("act", lambda bk=bk: nc.scalar.copy(out=stg[:], in_=bk[0:NE, :]), reads=[BKb], writes=[STG])
                        S.op("sp", lambda t0=t0: nc.sync.dma_start(out=affT_d[:, t0 * 128:(t0 + 4) * 128], in_=stg[:]),
                             reads=[STG], writes=[AFT], dma=STG)
                    S.barrier()
                    if stop == "B1":
                        return nc
                    S.op("pool", lambda: nc.gpsimd.collective_compute("AllGather", ALU.bypass, replica_groups=[list(range(NCORE))],
                                                                       ins=[affT_d[:, :]], outs=[gath_d[:, :]]),
                         reads=[AFT], writes=[GTH], dma=GTH, inc=1)
                    S.op("sp", lambda: nc.sync.dma_start(out=G[:], in_=gath_d[:, :]), reads=[GTH], writes=[GB], dma=GB)
                    if stop == "B2":
                        S.barrier()
                        return nc
                    lo, hi, mid, cntv, ge, tmp = bis[:, 0:2], bis[:, 2:4], bis[:, 4:6], bis[:, 6:8], bis[:, 8:10], bis[:, 10:12]
                    S.op("dve", lambda: nc.vector.memset(lo, 0.0), writes=[BIS])
                    S.op("dve", lambda: nc.vector.memset(hi, 1.0001), writes=[BIS])
                    grp_cols = [(0, nP * S_LEN), (nP * S_LEN, NT)]
                    for it in range(34):
                        S.op("dve", lambda: nc.vector.tensor_tensor(out=mid, in0=lo, in1=hi, op=ALU.add), writes=[BIS])
                        S.op("dve", lambda: nc.vector.tensor_scalar(out=mid, in0=mid, scalar1=0.5, scalar2=None, op0=ALU.mult), writes=[BIS])
                        for gi, (c0, c1) in enumerate(grp_cols):
                            S.op("dve", lambda gi=gi, c0=c0, c1=c1: nc.vector.tensor_scalar(
                                out=cmpj[:, 0:c1 - c0], in0=G[:, c0:c1], scalar1=bis[:, 4 + gi:5 + gi], scalar2=0.0,
                                op0=ALU.is_ge, op1=ALU.add, accum_out=bis[:, 6 + gi:7 + gi]), reads=[GB], writes=[CMPJ, BIS])
                        bk, BKb = nextbank()
                        S.op("pe", lambda bk=bk: nc.tensor.matmul(bk[:, 0:2], lhsT=gsel, rhs=cntv, start=True, stop=True),
                             reads=[BIS, CSTF], writes=[BKb])
                        S.op("dve", lambda bk=bk: nc.vector.tensor_tensor(out=ge, in0=bk[:, 0:2], in1=capv[:], op=ALU.is_ge),
                             reads=[BKb, CAPV], writes=[BIS])
                        S.op("dve", lambda: nc.vector.tensor_tensor(out=tmp, in0=mid, in1=lo, op=ALU.subtract), writes=[BIS])
                        S.op("dve", lambda: nc.vector.tensor_tensor(out=tmp, in0=tmp, in1=ge, op=ALU.mult), writes=[BIS])
                        S.op("dve", lambda: nc.vector.tensor_tensor(out=lo, in0=lo, in1=tmp, op=ALU.add), writes=[BIS])
                        S.op("dve", lambda: nc.vector.tensor_tensor(out=tmp, in0=hi, in1=mid, op=ALU.subtract), writes=[BIS])
                        S.op("dve", lambda: nc.vector.tensor_tensor(out=tmp, in0=tmp, in1=ge, op=ALU.mult), writes=[BIS])
                        S.op("dve", lambda: nc.vector.tensor_tensor(out=hi, in0=mid, in1=tmp, op=ALU.add), writes=[BIS])
                    if dbg == "Bonly":
                        print("nops at B3", S.nops)
                    if stop == "B3":
                        S.barrier()
                        return nc
                    S.op("dve", lambda: nc.vector.memset(thb[:], 1.0), writes=[THB])
                    bk, BKb = nextbank()
                    for gi in range(2):
                        S.op("dve", lambda gi=gi: nc.vector.tensor_scalar(out=thb[:, gi, :], in0=thb[:, gi, :], scalar1=bis[0:NE, gi:gi + 1],
                                                                           scalar2=None, op0=ALU.mult), reads=[BIS], writes=[THB])
                        S.op("pe", lambda gi=gi, bk=bk: nc.tensor.matmul(bk[:, gi * NE:(gi + 1) * NE], lhsT=thb[:, gi, :], rhs=i16, start=True, stop=True),
                             reads=[THB, CSTF], writes=[BKb])
                    S.op("act", lambda bk=bk: nc.scalar.copy(out=thr[:], in_=bk[:, 0:2 * NE].rearrange("p (g e) -> p g e", g=2)), reads=[BKb], writes=[THR])
                    for gt in range(NTILE):
                        gi = 0 if gt < nPT else 1
                        S.op("dve", lambda gt=gt, gi=gi: nc.vector.tensor_tensor(out=Mk[:, gt, :], in0=aff_all[:, gt, :], in1=thr[:, gi, :], op=ALU.is_ge),
                             reads=[AFF, THR], writes=[MK])
                    Mkf = Mk[:, :, :].rearrange("p t e -> p (t e)")
                    S.op("dve", lambda: nc.vector.tensor_copy(out=Mkb[:], in_=Mkf), reads=[MK], writes=[MKB])
                    ncol = NTILE * NE
                    for c0 in range(0, ncol, 512):
                        n = min(512, ncol - c0)
                        for lhs, dst, DST in ((Ub, pre, PRE), (onesb, tot, TOT)):
                            bk, BKb = nextbank()
                            S.op("pe", lambda bk=bk, lhs=lhs, c0=c0, n=n: nc.tensor.matmul(bk[:, 0:n], lhsT=lhs, rhs=Mkb[:, c0:c0 + n], start=True, stop=True),
                                 reads=[MKB, CB], writes=[BKb])
                            S.op("act", lambda bk=bk, dst=dst, c0=c0, n=n: nc.scalar.copy(
                                out=dst[:, :, :].rearrange("p t e -> p (t e)")[:, c0:c0 + n], in_=bk[:, 0:n]), reads=[BKb], writes=[DST])
                    hsA, hsB = cum, hs2
                    S.op("dve", lambda: nc.vector.tensor_copy(out=hsA[:], in_=Mk[:]), reads=[MK], writes=[CUM])
                    HSB = {id(cum): CUM, id(hs2): HS2}
                    sft = 1
                    while sft < NTILE:
                        S.op("dve", lambda hsA=hsA, hsB=hsB, sft=sft: nc.vector.tensor_tensor(out=hsB[:, sft:, :], in0=hsA[:, sft:, :], in1=hsA[:, :NTILE - sft, :], op=ALU.add),
                             reads=[HSB[id(hsA)]], writes=[HSB[id(hsB)]])
                        S.op("dve", lambda hsA=hsA, hsB=hsB, sft=sft: nc.vector.tensor_copy(out=hsB[:, :sft, :], in_=hsA[:, :sft, :]),
                             reads=[HSB[id(hsA)]], writes=[HSB[id(hsB)]])
                        hsA, hsB = hsB, hsA
                        sft *= 2
                    S.op("dve", lambda: nc.vector.memset(onesf[:], 1.0), writes=[ONF])
                    hf = hsA[:, :, :].rearrange("p t e -> p (t e)")
                    cumf = cum2[:, :, :].rearrange("p t e -> p (t e)")
                    for c0 in range(0, ncol, 512):
                        n = min(512, ncol - c0)
                        bk, BKb = nextbank()
                        S.op("pe", lambda bk=bk, c0=c0, n=n: nc.tensor.matmul(bk[:, 0:n], lhsT=onesf[:], rhs=hf[:, c0:c0 + n], start=True, stop=True),
                             reads=[HSB[id(hsA)], ONF], writes=[BKb])
                        S.op("act", lambda bk=bk, c0=c0, n=n: nc.scalar.copy(out=cumf[:, c0:c0 + n], in_=bk[:, 0:n]), reads=[BKb], writes=[CUM2])
                    pf = pre[:, :, :].rearrange("p t e -> p (t e)")
                    S.op("dve", lambda: nc.vector.tensor_tensor(out=pf, in0=pf, in1=cum2[:, :, :].rearrange("p t e -> p (t e)"), op=ALU.add),
                         reads=[CUM2], writes=[PRE])
                    S.op("dve", lambda: nc.vector.tensor_tensor(out=pf, in0=pf, in1=tot[:, :, :].rearrange("p t e -> p (t e)"), op=ALU.subtract),
                         reads=[TOT], writes=[PRE])
                    S.op("dve", lambda: nc.vector.tensor_scalar(out=Mkf, in0=Mkf, scalar1=-1.0e6, scalar2=1.0e6, op0=ALU.mult, op1=ALU.add), writes=[MK])
                    S.op("dve", lambda: nc.vector.tensor_tensor(out=pf, in0=pf, in1=Mkf, op=ALU.add), reads=[MK], writes=[PRE])
                    S.op("dve", lambda: nc.vector.tensor_copy(out=idxi[:, :, :].rearrange("p t e -> p (t e)"), in_=pf), reads=[PRE], writes=[IDX])
                    if dbg == "Bonly":
                        print("nops at B4", S.nops)
                    if stop == "B4":
                        S.barrier()
                        return nc
                    for gt in range(NTILE):
                        rl, RLb = recl[gt % 3], RECL[gt % 3]
                        S.op("sp", lambda gt=gt, rl=rl: nc.sync.dma_start(out=rl[:], in_=h2rec[gt * 128:(gt + 1) * 128, :]),
                             reads=[H2R], writes=[RLb], dma=RLb)
                        for e in range(NE):
                            S.op("pool", lambda gt=gt, e=e, rl=rl: nc.gpsimd.indirect_dma_start(
                                out=xs_d[e][:, :], out_offset=bass.IndirectOffsetOnAxis(ap=idxi[:, gt, e:e + 1], axis=0),
                                in_=rl[:], in_offset=None, bounds_check=bc_slots, oob_is_err=False),
                                reads=[RLb, IDX, XSD], dma=RLb)
                    if dbg is not None and l == 0:
                        S.op("sp", lambda: nc.sync.dma_start(out=dbg_out[:, 0:16], in_=bis[:]), reads=[BIS], writes=[DBG], dma=DBG)
                        S.op("sp", lambda: nc.sync.dma_start(out=dbg_out[:, 16:48], in_=thr[:, :, :].rearrange("p g e -> p (g e)")), reads=[THR], writes=[DBG], dma=DBG)
                        S.op("sp", lambda: nc.sync.dma_start(out=dbg_out[:, 48:64], in_=tot[:, NTILE - 1, :]), reads=[TOT], writes=[DBG], dma=DBG)
                    S.barrier()
                    S.release_since(mk_b)
                if dbg == "Bonly":
                    for k, e in enumerate((0, 5)):
                        S.op("sp", lambda k=k, e=e: nc.sync.dma_start(out=y_out[k * cap_slots:(k + 1) * cap_slots, :], in_=xs_d[e][:, 0:D]), writes=[DBG], dma=DBG)
                        S.op("sp", lambda k=k, e=e: nc.sync.dma_start(out=y1_out[k * cap_slots:(k + 1) * cap_slots, 0:32], in_=xs_d[e][:, D:D + 32]), writes=[DBG], dma=DBG)
                    S.barrier()
                    return nc
                if dbg == "B":
                    return nc
                mk_c = S.mark()
                with ExitStack() as lm:
                    xsfs = [sb(lm, "xsf%d" % i, [128, RECW]) for i in range(2)]; XSFS = [S.buf("xsf%d" % i) for i in range(2)]
                    xsbs = [sb(lm, "xsb%d" % i, [128, D], BF16) for i in range(2)]; XSBS = [S.buf("xsb%d" % i) for i in range(2)]
                    xsT = sb(lm, "xsT", [128, 8, cap_slots], BF16); XST = S.buf("xsT")
                    gT = sb(lm, "gT", [128, 16, cap_slots], BF16); GT = S.buf("gT")
                    gate_all = sb(lm, "gate_all", [128, CAPT]); GA = S.buf("gate_all")
                    tok_i = sb(lm, "tok_i", [128, CAPT], I32); TK = S.buf("tok_i")
                    wgu = [sb(lm, "wgu%d" % i, [128, 8, 2, 512], BF16) for i in range(2)]; WGU = [S.buf("wgu%d" % i) for i in range(2)]
                    wd = sb(lm, "wd", [128, 16, D], BF16); WD = S.buf("wd")
                    sil = [sb(lm, "sil%d" % i, [128, 512]) for i in range(2)]; SIL = [S.buf("sil%d" % i) for i in range(2)]
                    y_sbs = [sb(lm, "y_sb%d" % i, [128, D]) for i in range(2)]; YSS = [S.buf("y_sb%d" % i) for i in range(2)]
                    sgs = [(a, min(512, cap_slots - a)) for a in range(0, cap_slots, 512)]
                    wq = [0]
                    for e in range(NE):
                        for stile in range(CAPT):
                            xsf, XSF = xsfs[stile % 2], XSFS[stile % 2]
                            xsb, XSBb = xsbs[stile % 2], XSBS[stile % 2]
                            S.op("sp", lambda e=e, stile=stile, xsf=xsf: nc.sync.dma_start(out=xsf[:], in_=xs_d[e][stile * 128:(stile + 1) * 128, :]),
                                 reads=[XSD], writes=[XSF], dma=XSF)
                            S.op("act", lambda xsf=xsf, xsb=xsb: nc.scalar.copy(out=xsb[:], in_=xsf[:, 0:D]), reads=[XSF], writes=[XSBb])
                            S.op("dve", lambda e=e, stile=stile, xsf=xsf: nc.vector.tensor_copy(out=gate_all[:, stile:stile + 1], in_=xsf[:, D + e:D + e + 1]),
                                 reads=[XSF], writes=[GA])
                            S.op("dve", lambda stile=stile, xsf=xsf: nc.vector.tensor_copy(out=tok_i[:, stile:stile + 1], in_=xsf[:, D + NE:D + NE + 1]),
                                 reads=[XSF], writes=[TK])
                            for kc in range(8):
                                S.op("pe", lambda kc=kc, xsb=xsb: nc.tensor.transpose(out=bankb[:, kc * 128:(kc + 1) * 128], in_=xsb[:, kc * 128:(kc + 1) * 128],
                                                                             identity=identb), reads=[XSBb, CB], writes=[BKB])
                            S.op("act", lambda stile=stile: nc.scalar.copy(out=xsT[:, :, stile * 128:(stile + 1) * 128],
                                                                           in_=bankb[:, :].rearrange("p (k t) -> p k t", k=8)), reads=[BKB], writes=[XST])
                        for fq in range(4):
                            wg, WGb = wgu[wq[0] % 2], WGU[wq[0] % 2]
                            wq[0] += 1
                            for t2, wsrc in enumerate((w_gate, w_up)):
                                S.op("pool", lambda t2=t2, wsrc=wsrc, wg=wg, e=e, fq=fq: nc.gpsimd.dma_start(
                                    out=wg[:, :, t2, :], in_=wsrc[l, e].rearrange("(kc k) f -> k kc f", k=128)[:, :, fq * 512:(fq + 1) * 512]),
                                    writes=[WGb], dma=WGb)
                            for fc4 in range(4):
                                fc = fq * 4 + fc4
                                for si, (a0, an) in enumerate(sgs):
                                    bka, BKa = nextbank()
                                    bku, BKu = nextbank()
                                    for t2, bk in ((0, bka), (1, bku)):
                                        for kc in range(8):
                                            S.op("pe", lambda kc=kc, t2=t2, bk=bk, wg=wg, fc4=fc4, a0=a0, an=an: nc.tensor.matmul(
                                                bk[:, 0:an], lhsT=wg[:, kc, t2, fc4 * 128:(fc4 + 1) * 128], rhs=xsT[:, kc, a0:a0 + an],
                                                start=(kc == 0), stop=(kc == 7)), reads=[WGb, XST], writes=[BKa if t2 == 0 else BKu])
                                    sl, SLb = sil[si % 2], SIL[si % 2]
                                    S.op("act", lambda bka=bka, sl=sl, an=an: nc.scalar.activation(out=sl[:, 0:an], in_=bka[:, 0:an], func=AF.Silu),
                                         reads=[BKa], writes=[SLb])
                                    S.op("dve", lambda bku=bku, sl=sl, fc=fc, a0=a0, an=an: nc.vector.tensor_tensor(
                                        out=gT[:, fc, a0:a0 + an], in0=sl[:, 0:an], in1=bku[:, 0:an], op=ALU.mult), reads=[SLb, BKu], writes=[GT])
                        S.op("pool", lambda e=e: nc.gpsimd.dma_start(out=wd[:, 0:8, :], in_=w_down[l, e].rearrange("(fc f) d -> f fc d", f=128)[:, 0:8, :]),
                             writes=[WD], dma=WD)
                        S.op("pool", lambda e=e: nc.gpsimd.dma_start(out=wd[:, 8:16, :], in_=w_down[l, e].rearrange("(fc f) d -> f fc d", f=128)[:, 8:16, :]),
                             writes=[WD], dma=WD)
                        for stile in range(CAPT):
                            y_sb, YS = y_sbs[stile % 2], YSS[stile % 2]
                            for dh in range(2):
                                bk, BKb = nextbank()
                                for fc in range(16):
                                    S.op("pe", lambda fc=fc, bk=bk, stile=stile, dh=dh: nc.tensor.matmul(
                                        bk[:, :], lhsT=gT[:, fc, stile * 128:(stile + 1) * 128], rhs=wd[:, fc, dh * 512:(dh + 1) * 512],
                                        start=(fc == 0), stop=(fc == 15)), reads=[GT, WD], writes=[BKb])
                                S.op("dve", lambda bk=bk, stile=stile, dh=dh, y_sb=y_sb: nc.vector.tensor_scalar(
                                    out=y_sb[:, dh * 512:(dh + 1) * 512], in0=bk[:, :], scalar1=gate_all[:, stile:stile + 1], scalar2=None, op0=ALU.mult),
                                    reads=[BKb, GA], writes=[YS])
                            S.op("pool", lambda stile=stile, y_sb=y_sb: nc.gpsimd.indirect_dma_start(
                                out=xbuf[:, :], out_offset=bass.IndirectOffsetOnAxis(ap=tok_i[:, stile:stile + 1], axis=0),
                                in_=y_sb[:], in_offset=None, bounds_check=bc_rows, oob_is_err=True, compute_op=ALU.add),
                                reads=[YS, TK], writes=[XBALL], dma=YS)
                    S.barrier()
                    S.release_since(mk_c)
                if dbg == "C":
                    for q0 in range(0, NT, 2048):
                        S.op("sp", lambda q0=q0: nc.sync.dma_start(out=y_out[q0:q0 + 2048, :], in_=xbuf[q0:q0 + 2048, :]), writes=[DBG], dma=DBG)
                    S.barrier()
                    return nc
            with ExitStack() as lf:
                gf = sb(lf, "gf", [128, D]); GF = S.buf("gf")
                xf = [sb(lf, "xf%d" % i, [128, D]) for i in range(2)]; XF = [S.buf("xf%d" % i) for i in range(2)]
                yf = [sb(lf, "yf%d" % i, [128, D]) for i in range(2)]; YF = [S.buf("yf%d" % i) for i in range(2)]
                junk2 = sb(lf, "junk2", [128, D], BF16); J2 = S.buf("junk2")
                YO = S.buf("yout")
                S.op("sp", lambda: nc.sync.dma_start(out=gf[:], in_=final_g[0:1, :].to_broadcast([128, D])), writes=[GF], dma=GF)
                for gt in range(NTILE):
                    xx, XXb = xf[gt % 2], XF[gt % 2]
                    yy, YYb = yf[gt % 2], YF[gt % 2]
                    ss, rs = small[:, 8:9], small[:, 9:10]
                    S.op("sp", lambda gt=gt, xx=xx: nc.sync.dma_start(out=xx[:], in_=xbuf[gt * 128:(gt + 1) * 128, :]), writes=[XXb], dma=XXb)
                    S.op("act", lambda xx=xx: nc.scalar.activation(out=junk2[:], in_=xx[:], func=AF.Square, accum_out=ss),
                         reads=[XXb], writes=[J2, SM[8]])
                    rstd_ops(ss, rs, SM[8], SM[9])
                    S.op("dve", lambda xx=xx, yy=yy: nc.vector.scalar_tensor_tensor(out=yy[:], in0=xx[:], scalar=rs, in1=gf[:], op0=ALU.mult, op1=ALU.mult),
                         reads=[XXb, SM[9], GF], writes=[YYb])
                    S.op("sp", lambda gt=gt, yy=yy: nc.sync.dma_start(out=y_out[gt * 128:(gt + 1) * 128, :], in_=yy[:]), reads=[YYb], writes=[YO], dma=YYb)
            S.barrier()

      except StopBuild:
          S.stop_at = None
          S.barrier()
    return nc


def run(inputs, nP, nS, cap_slots, dbg=None):
    nc = build(nP, nS, cap_slots, dbg)
    cstf, cstb = make_consts()
    NT = (nP + nS) * S_LEN
    xs_init = np.zeros((cap_slots, RECW), np.float32)
    xs_init[:, D + NE] = NT + (np.arange(cap_slots) % 128)
    capv = np.zeros((128, 2), np.float32)
    capv[:, 0] = 2 * (nP * NCORE * S_LEN) // NE
    capv[:, 1] = 2 * (nS * NCORE * S_LEN) // NE
    f = lambda a: np.ascontiguousarray(np.asarray(a, dtype=np.float32))
    shared = {k: f(inputs[k]) for k in ("norm1_g", "w_in", "conv_w", "w_conv_out", "w_attn_out", "w_o", "norm2_g",
                                        "w_router", "w_gate", "w_up", "w_down")}
    shared["final_g"] = f(inputs["final_g"]).reshape(1, D)
    shared.update(cstf=cstf, cstb=cstb, xs_init=xs_init, capv=capv)
    xp = f(inputs["x_prompt"])
    xs = f(inputs["x_sample"])
    in_maps = []
    for c in range(NCORE):
        xi = np.concatenate([xp[c * nP:(c + 1) * nP].reshape(-1, D), xs[c * nS:(c + 1) * nS].reshape(-1, D)], 0)
        m = dict(shared)
        m["x_in"] = np.ascontiguousarray(xi)
        in_maps.append(m)
    res = run_bass_kernel_spmd(nc, in_maps, core_ids=list(range(NCORE)))
    if dbg is not None:
        return res
    yp = np.zeros_like(xp)
    ys = np.zeros_like(xs)
    for c in range(NCORE):
        y = res.results[c]["y"]
        yp[c * nP:(c + 1) * nP] = y[:nP * S_LEN].reshape(nP, S_LEN, D)
        ys[c * nS:(c + 1) * nS] = y[nP * S_LEN:].reshape(nS, S_LEN, D)
    return yp, ys


def kernel(**inputs):
    yp, ys = run(inputs, 4, 2, 1792)
    return (yp, ys)
```
